# Optimizing a Trainium2 kernel written in Bass

```python
import jax
import jax.numpy as jnp
from jax import lax
import numpy as np

D_MODEL = 4096
BATCH = 4
SEQ = 4096
DEPTH = 2

GRID_W = 64
CTX_LEN = 256
N_MOD = 6
EPS = 1e-6
ROPE_BASE = 10000.0

NA_HEADS = 16
NA_HEAD_DIM = 128
NA_WIN_ROWS = 8
NA_WIN_COLS = 16

GLA_HEADS = 4
GLA_DK = 128
GLA_DV = 256
GLA_LOWRANK = 16
GLA_NORMALIZER = 16.0
GLA_CHUNK = 64

LRU_WIDTH = 1024
LRU_BLOCKS = 8
LRU_BW = LRU_WIDTH // LRU_BLOCKS
LRU_CONV = 4
LRU_C = 8.0

D_FF_DENSE = 8192
N_EXPERTS = 8
TOP_K = 2
D_FF_EXPERT = 4096

NA_W = NA_HEADS * NA_HEAD_DIM
GLA_KW = GLA_HEADS * GLA_DK
GLA_VW = GLA_HEADS * GLA_DV
MIX_W = NA_W + GLA_VW + LRU_WIDTH
IN_SPLITS = (NA_W,) * 3 + (GLA_KW,) * 2 + (GLA_VW,) * 2 + (GLA_LOWRANK,) * 2 + (LRU_WIDTH,) * 2
IN_WIDTH = sum(IN_SPLITS)
IN_SPLIT_POINTS = tuple(int(p) for p in np.cumsum(IN_SPLITS)[:-1])

kernel_name = 'hybrid_na_gla_rglru_moe_dit_block'


def rmsnorm(x, w):
    xf = x.astype(jnp.float32)
    y = xf * lax.rsqrt(jnp.mean(xf * xf, axis=-1, keepdims=True) + EPS)
    return (y * w.astype(jnp.float32)).astype(x.dtype)


def to_heads(t, n_heads):
    b, s, _ = t.shape
    return t.reshape(b, s, n_heads, -1).transpose(0, 2, 1, 3)


def from_heads(t):
    b, h, s, d = t.shape
    return t.transpose(0, 2, 1, 3).reshape(b, s, h * d)


def axial_rope(t, row, col):
    half = t.shape[-1] // 2
    quarter = half // 2
    inv_freq = ROPE_BASE ** (-jnp.arange(quarter, dtype=jnp.float32) / quarter)

    def rotate(u, p):
        ang = p.astype(jnp.float32)[:, None] * inv_freq
        cos, sin = jnp.cos(ang).astype(u.dtype), jnp.sin(ang).astype(u.dtype)
        u1, u2 = u[..., :quarter], u[..., quarter:]
        return jnp.concatenate([u1 * cos - u2 * sin, u1 * sin + u2 * cos], axis=-1)

    return jnp.concatenate([rotate(t[..., :half], row), rotate(t[..., half:], col)], axis=-1)


def dense_attention(q, k, v):
    s = jnp.einsum('bhqd,bhkd->bhqk', q, k).astype(jnp.float32)
    p = jax.nn.softmax(s, axis=-1).astype(v.dtype)
    return jnp.einsum('bhqk,bhkd->bhqd', p, v)


def neighbourhood_attention(q, k, v, k_ctx, v_ctx, rpb):
    b, h, n, d = q.shape
    rows = n // GRID_W
    wr = min(NA_WIN_ROWS, rows)
    wc = NA_WIN_COLS
    qg = q.reshape(b, h, rows, GRID_W, d)
    kg = k.reshape(b, h, rows, GRID_W, d)
    vg = v.reshape(b, h, rows, GRID_W, d)
    cols = jnp.arange(GRID_W)
    col_start = jnp.clip(cols - wc // 2, 0, GRID_W - wc)
    col_idx = col_start[:, None] + jnp.arange(wc)[None, :]
    dc = col_idx - cols[:, None] + (NA_WIN_COLS - 1)

    def row_block(r):
        rs = jnp.clip(r - wr // 2, 0, rows - wr)
        q_r = lax.dynamic_index_in_dim(qg, r, axis=2, keepdims=False)
        k_rows = lax.dynamic_slice_in_dim(kg, rs, wr, axis=2)
        v_rows = lax.dynamic_slice_in_dim(vg, rs, wr, axis=2)
        k_win = k_rows[:, :, :, col_idx]
        v_win = v_rows[:, :, :, col_idx]
        dr = rs + jnp.arange(wr) - r + (NA_WIN_ROWS - 1)
        bias = rpb[:, dr[:, None, None], dc[None, :, :]]
        s_win = jnp.einsum('bhcd,bhicjd->bhcij', q_r, k_win).astype(jnp.float32)
        s_win = s_win + bias.transpose(0, 2, 1, 3).astype(jnp.float32)[None]
        s_ctx = jnp.einsum('bhcd,bhld->bhcl', q_r, k_ctx).astype(jnp.float32)
        s = jnp.concatenate([s_win.reshape(b, h, GRID_W, wr * wc), s_ctx], axis=-1)
        p = jax.nn.softmax(s, axis=-1).astype(v.dtype)
        p_win = p[..., :wr * wc].reshape(b, h, GRID_W, wr, wc)
        p_ctx = p[..., wr * wc:]
        return (jnp.einsum('bhcij,bhicjd->bhcd', p_win, v_win)
                + jnp.einsum('bhcl,bhld->bhcd', p_ctx, v_ctx))

    o = lax.map(row_block, jnp.arange(rows))
    return o.transpose(1, 2, 0, 3, 4).reshape(b, h, n, d)


def gla_inputs(q, k, v, lr_f, lr_b, wg, bg, pos):
    f32 = jnp.float32
    q = to_heads(q, GLA_HEADS).astype(f32) * GLA_DK ** -0.5
    k = to_heads(k, GLA_HEADS).astype(f32)
    v = to_heads(v, GLA_HEADS).astype(f32)
    if pos is not None:
        q = axial_rope(q, pos[0], pos[1])
        k = axial_rope(k, pos[0], pos[1])
    log_f = to_heads(jax.nn.log_sigmoid((lr_f @ wg[0] + bg[0]).astype(f32)) / GLA_NORMALIZER, GLA_HEADS)
    log_b = to_heads(jax.nn.log_sigmoid((lr_b @ wg[1] + bg[1]).astype(f32)) / GLA_NORMALIZER, GLA_HEADS)
    return q, k, v, log_f, log_b


def gla_chunked(q, k, v, logg, h0):
    b_, h_, t_, dk = q.shape
    dv = v.shape[-1]
    nc = t_ // GLA_CHUNK
    q = q.reshape(b_, h_, nc, GLA_CHUNK, dk)
    k = k.reshape(b_, h_, nc, GLA_CHUNK, dk)
    logg = logg.reshape(b_, h_, nc, GLA_CHUNK, dk)
    v = v.reshape(b_, h_, nc, GLA_CHUNK, dv)
    b = jnp.cumsum(logg, axis=3)
    b_end = b[:, :, :, -1:, :]
    q_dec = q * jnp.exp(b)
    k_inv = k * jnp.exp(-b)
    k_end = k * jnp.exp(b_end - b)
    lower = jnp.tril(jnp.ones((GLA_CHUNK, GLA_CHUNK), dtype=bool))
    att = jnp.where(lower, jnp.einsum('bhnik,bhnjk->bhnij', q_dec, k_inv), 0.0)
    o_intra = jnp.einsum('bhnij,bhnjv->bhniv', att, v)
    d_state = jnp.einsum('bhnjk,bhnjv->bhnkv', k_end, v)
    decay = jnp.exp(b_end[:, :, :, 0, :])

    def step(state, inp):
        dec, ds = inp
        return dec[..., None] * state + ds, state

    s_final, s_prev = lax.scan(step, h0, (jnp.moveaxis(decay, 2, 0), jnp.moveaxis(d_state, 2, 0)))
    o_inter = jnp.einsum('bhnik,bhnkv->bhniv', q_dec, jnp.moveaxis(s_prev, 0, 2))
    return (o_intra + o_inter).reshape(b_, h_, t_, dv), s_final


def gla_final_state(k, v, logg):
    b = jnp.cumsum(logg, axis=2)
    return jnp.einsum('bhtk,bhtv->bhkv', k * jnp.exp(b[:, :, -1:, :] - b), v)


def gla_output(o, g, norm_w):
    b, h, t, dv = o.shape
    o = rmsnorm(o.transpose(0, 2, 1, 3), norm_w).reshape(b, t, h * dv)
    return o.astype(g.dtype) * jax.nn.silu(g)


def conv_centred(x, w, b):
    kw = w.shape[0]
    y = lax.conv_general_dilated(x, w[:, None, :], window_strides=(1,),
                                 padding=[(kw // 2, kw - 1 - kw // 2)],
                                 dimension_numbers=('NWC', 'WIO', 'NWC'),
                                 feature_group_count=x.shape[-1])
    return y + b


def rglru_coeffs(xc, wa, ba, wx, bx, lam):
    b, t, _ = xc.shape
    xb = xc.reshape(b, t, LRU_BLOCKS, LRU_BW)
    r = jax.nn.sigmoid(jnp.einsum('btnc,ncd->btnd', xb, wa).reshape(b, t, LRU_WIDTH) + ba)
    i = jax.nn.sigmoid(jnp.einsum('btnc,ncd->btnd', xb, wx).reshape(b, t, LRU_WIDTH) + bx)
    log_a = -LRU_C * r * jax.nn.softplus(-lam)
    return jnp.exp(log_a), jnp.sqrt(-jnp.expm1(2.0 * log_a)) * (i * xc)


def rglru_directions(rx, conv_w, conv_b, wa, ba, wx, bx, lam):
    xc = conv_centred(rx, conv_w, conv_b).astype(jnp.float32)
    fwd = rglru_coeffs(xc, wa[0], ba[0], wx[0], bx[0], lam[0])
    bwd = rglru_coeffs(xc, wa[1], ba[1], wx[1], bx[1], lam[1])
    return fwd, bwd


def linear_scan(a, b, h0):
    def combine(left, right):
        return left[0] * right[0], right[0] * left[1] + right[1]
    acc_a, acc_b = lax.associative_scan(combine, (a, b), axis=1)
    return acc_a * h0[:, None, :] + acc_b


def token_mixer(h_l, h_c, row, col, w_in, rpb, gla_wg, gla_bg, gla_norm, conv_w, conv_b,
                lru_wa, lru_ba, lru_wx, lru_bx, lru_lam, w_out, with_ctx_out):
    (nq_l, nk_l, nv_l, gq_l, gk_l, gv_l, gg_l, glf_l, glb_l, rx_l, rg_l) = jnp.split(h_l @ w_in, IN_SPLIT_POINTS, axis=-1)
    (nq_c, nk_c, nv_c, gq_c, gk_c, gv_c, gg_c, glf_c, glb_c, rx_c, rg_c) = jnp.split(h_c @ w_in, IN_SPLIT_POINTS, axis=-1)
    flip2 = lambda t: jnp.flip(t, axis=2)
    flip1 = lambda t: jnp.flip(t, axis=1)

    na_scale = NA_HEAD_DIM ** -0.5
    nk_c, nv_c = to_heads(nk_c, NA_HEADS), to_heads(nv_c, NA_HEADS)
    na_l = from_heads(neighbourhood_attention(to_heads(nq_l, NA_HEADS) * na_scale, to_heads(nk_l, NA_HEADS),
                                              to_heads(nv_l, NA_HEADS), nk_c, nv_c, rpb))

    q_l, k_l, v_l, lf_l, lb_l = gla_inputs(gq_l, gk_l, gv_l, glf_l, glb_l, gla_wg, gla_bg, (row, col))
    q_c, k_c, v_c, lf_c, lb_c = gla_inputs(gq_c, gk_c, gv_c, glf_c, glb_c, gla_wg, gla_bg, None)
    if with_ctx_out:
        zero_s = jnp.zeros(k_c.shape[:2] + (GLA_DK, GLA_DV), jnp.float32)
        oc_f, s_f = gla_chunked(q_c, k_c, v_c, lf_c, zero_s)
        oc_b, s_b = gla_chunked(flip2(q_c), flip2(k_c), flip2(v_c), flip2(lb_c), zero_s)
        gla_c = gla_output(oc_f + flip2(oc_b), gg_c, gla_norm)
    else:
        s_f = gla_final_state(k_c, v_c, lf_c)
        s_b = gla_final_state(flip2(k_c), flip2(v_c), flip2(lb_c))
    ol_f, _ = gla_chunked(q_l, k_l, v_l, lf_l, s_f)
    ol_b, _ = gla_chunked(flip2(q_l), flip2(k_l), flip2(v_l), flip2(lb_l), s_b)
    gla_l = gla_output(ol_f + flip2(ol_b), gg_l, gla_norm)

    (ca_f, cb_f), (ca_b, cb_b) = rglru_directions(rx_c, conv_w, conv_b, lru_wa, lru_ba, lru_wx, lru_bx, lru_lam)
    zero_h = jnp.zeros((rx_c.shape[0], LRU_WIDTH), jnp.float32)
    hc_f = linear_scan(ca_f, cb_f, zero_h)
    hc_b = flip1(linear_scan(flip1(ca_b), flip1(cb_b), zero_h))
    (la_f, lb_f), (la_b, lb_b) = rglru_directions(rx_l, conv_w, conv_b, lru_wa, lru_ba, lru_wx, lru_bx, lru_lam)
    hl_f = linear_scan(la_f, lb_f, hc_f[:, -1])
    hl_b = flip1(linear_scan(flip1(la_b), flip1(lb_b), hc_b[:, 0]))
    lru_l = (hl_f + hl_b).astype(h_l.dtype) * jax.nn.gelu(rg_l)

    out_l = jnp.concatenate([na_l, gla_l, lru_l], axis=-1) @ w_out
    if not with_ctx_out:
        return out_l, None
    na_c = from_heads(dense_attention(to_heads(nq_c, NA_HEADS) * na_scale, nk_c, nv_c))
    lru_c = (hc_f + hc_b).astype(h_c.dtype) * jax.nn.gelu(rg_c)
    out_c = jnp.concatenate([na_c, gla_c, lru_c], axis=-1) @ w_out
    return out_l, out_c


def swiglu(h, w1, w3, w2):
    return (jax.nn.silu(h @ w1) * (h @ w3)) @ w2


def moe_swiglu(h, router, w1, w3, w2):
    probs = jax.nn.softmax((h @ router).astype(jnp.float32), axis=-1)
    top_p, top_i = lax.top_k(probs, TOP_K)
    top_p = top_p / jnp.sum(top_p, axis=-1, keepdims=True)
    gates = jnp.sum(jax.nn.one_hot(top_i, N_EXPERTS, dtype=jnp.float32) * top_p[..., None], axis=-2).astype(h.dtype)
    out = jnp.zeros_like(h)
    for e in range(N_EXPERTS):
        out = out + gates[..., e:e + 1] * swiglu(h, w1[e], w3[e], w2[e])
    return out


def channel_mixer(h, l, ffd_w1, ffd_w3, ffd_w2, router, moe_w1, moe_w3, moe_w2):
    j = l // 2
    if l % 2 == 0:
        return swiglu(h, ffd_w1[j], ffd_w3[j], ffd_w2[j])
    return moe_swiglu(h, router[j], moe_w1[j], moe_w3[j], moe_w2[j])


def setup_inputs(seed: int = 0) -> dict:
    key = jax.random.key(seed)
    ks = iter(jax.random.split(key, 40))
    f32 = jnp.float32

    def nrm(shape, scale):
        return jax.random.normal(next(ks), shape, f32) * scale

    def gain(shape):
        return 1.0 + nrm(shape, 0.02)

    n_dense = (DEPTH + 1) // 2
    n_moe = DEPTH // 2
    a_init = jax.random.uniform(next(ks), (DEPTH, 2, LRU_WIDTH), f32, 0.9, 0.999)
    return {
        'x': nrm((BATCH, SEQ, D_MODEL), 1.0),
        'c': nrm((BATCH, D_MODEL), 1.0),
        'ctx': nrm((BATCH, CTX_LEN, D_MODEL), 1.0),
        'c_ctx': nrm((D_MODEL,), 1.0),
        'w_mod': nrm((DEPTH, D_MODEL, N_MOD * D_MODEL), 0.5 * D_MODEL ** -0.5),
        'b_mod': nrm((DEPTH, N_MOD * D_MODEL), 0.02),
        'norm_mix': gain((DEPTH, D_MODEL)),
        'norm_ffn': gain((DEPTH, D_MODEL)),
        'w_in': nrm((DEPTH, D_MODEL, IN_WIDTH), D_MODEL ** -0.5),
        'na_rpb': nrm((DEPTH, NA_HEADS, 2 * NA_WIN_ROWS - 1, 2 * NA_WIN_COLS - 1), 0.1),
        'gla_wg': nrm((DEPTH, 2, GLA_LOWRANK, GLA_KW), GLA_LOWRANK ** -0.5),
        'gla_bg': nrm((DEPTH, 2, GLA_KW), 0.1),
        'gla_norm': gain((DEPTH, GLA_DV)),
        'conv_w': nrm((DEPTH, LRU_CONV, LRU_WIDTH), LRU_CONV ** -0.5),
        'conv_b': nrm((DEPTH, LRU_WIDTH), 0.02),
        'lru_wa': nrm((DEPTH, 2, LRU_BLOCKS, LRU_BW, LRU_BW), LRU_BW ** -0.5),
        'lru_ba': nrm((DEPTH, 2, LRU_WIDTH), 0.02),
        'lru_wx': nrm((DEPTH, 2, LRU_BLOCKS, LRU_BW, LRU_BW), LRU_BW ** -0.5),
        'lru_bx': nrm((DEPTH, 2, LRU_WIDTH), 0.02),
        'lru_lam': jnp.log(a_init) - jnp.log1p(-a_init),
        'w_out': nrm((DEPTH, MIX_W, D_MODEL), MIX_W ** -0.5),
        'ffd_w1': nrm((n_dense, D_MODEL, D_FF_DENSE), D_MODEL ** -0.5),
        'ffd_w3': nrm((n_dense, D_MODEL, D_FF_DENSE), D_MODEL ** -0.5),
        'ffd_w2': nrm((n_dense, D_FF_DENSE, D_MODEL), D_FF_DENSE ** -0.5),
        'router': nrm((n_moe, D_MODEL, N_EXPERTS), D_MODEL ** -0.5),
        'moe_w1': nrm((n_moe, N_EXPERTS, D_MODEL, D_FF_EXPERT), D_MODEL ** -0.5),
        'moe_w3': nrm((n_moe, N_EXPERTS, D_MODEL, D_FF_EXPERT), D_MODEL ** -0.5),
        'moe_w2': nrm((n_moe, N_EXPERTS, D_FF_EXPERT, D_MODEL), D_FF_EXPERT ** -0.5),
        'final_norm': gain((D_MODEL,)),
    }


def reference(x, c, ctx, c_ctx, w_mod, b_mod, norm_mix, norm_ffn, w_in, na_rpb,
              gla_wg, gla_bg, gla_norm, conv_w, conv_b, lru_wa, lru_ba, lru_wx, lru_bx,
              lru_lam, w_out, ffd_w1, ffd_w3, ffd_w2, router, moe_w1, moe_w3, moe_w2,
              final_norm):
    n_tok = x.shape[1]
    pos = jnp.arange(n_tok, dtype=jnp.int32)
    row, col = pos // GRID_W, pos % GRID_W
    silu_c = jax.nn.silu(c)
    silu_cc = jax.nn.silu(c_ctx)
    for l in range(DEPTH):
        last = l == DEPTH - 1
        mod_l = (silu_c @ w_mod[l] + b_mod[l]).reshape(x.shape[0], 1, N_MOD, D_MODEL)
        mod_c = (silu_cc @ w_mod[l] + b_mod[l]).reshape(1, 1, N_MOD, D_MODEL)
        sh1, sc1, g1, sh2, sc2, g2 = (mod_l[:, :, i] for i in range(N_MOD))
        csh1, csc1, cg1, csh2, csc2, cg2 = (mod_c[:, :, i] for i in range(N_MOD))

        h_l = rmsnorm(x, norm_mix[l]) * (1 + sc1) + sh1
        h_c = rmsnorm(ctx, norm_mix[l]) * (1 + csc1) + csh1
        o_l, o_c = token_mixer(h_l, h_c, row, col, w_in[l], na_rpb[l], gla_wg[l], gla_bg[l],
                               gla_norm[l], conv_w[l], conv_b[l], lru_wa[l], lru_ba[l],
                               lru_wx[l], lru_bx[l], lru_lam[l], w_out[l], not last)
        x = x + g1 * o_l
        h_l = rmsnorm(x, norm_ffn[l]) * (1 + sc2) + sh2
        x = x + g2 * channel_mixer(h_l, l, ffd_w1, ffd_w3, ffd_w2, router, moe_w1, moe_w3, moe_w2)
        if not last:
            ctx = ctx + cg1 * o_c
            h_c = rmsnorm(ctx, norm_ffn[l]) * (1 + csc2) + csh2
            ctx = ctx + cg2 * channel_mixer(h_c, l, ffd_w1, ffd_w3, ffd_w2, router, moe_w1, moe_w3, moe_w2)
    return rmsnorm(x, final_norm)
```

```python
import numpy as np
from contextlib import ExitStack
import concourse.bass as bass
import concourse.mybir as mybir
from concourse.bass_utils import run_bass_kernel_spmd

F32, BF16 = mybir.dt.float32, mybir.dt.bfloat16
AF = mybir.ActivationFunctionType
ALU = mybir.AluOpType
AX = mybir.AxisListType
EPS = 1e-6
NEG = -30000.0
NCORES = 8
RS_OVERLAP = False
SAME_ENGINE_SYNC = True


class Cfg:
    def __init__(s, D=4096, NROWS=64, CTX=256, FF=8192, FFE=4096):
        s.D = D; s.KD = D // 128; s.NROWS = NROWS; s.SEQ = 64 * NROWS; s.CTX = CTX
        s.TS = CTX + s.SEQ; s.CH = CTX // 2; s.LH = s.SEQ // 2; s.TL = s.CH + s.LH
        s.FF = FF; s.FS = FF // 8; s.FFE = FFE; s.MC = 6 * D // 8; s.MJ = s.MC // 128
        s.NCH = s.TS // 64; s.NCC = CTX // 64
        s.NFM = 11; s.FMW = 11 * 128; s.TMW = 768


FULL = Cfg()


class TR:
    NQ = 8

    def __init__(s, nc, es):
        s.nc = nc
        s.obj = {'pe': nc.tensor, 'act': nc.scalar, 'dve': nc.vector, 'pool': nc.gpsimd, 'sp': nc.sync}
        s.stream = {'pe': 'pe', 'act': 'act', 'dve': 'dve', 'pool': 'pool', 'sp': 'sp',
                    'gq': 'pool', 'aq': 'act', 'cc': 'pool'}
        s.issuer = {'pe': nc.tensor, 'act': nc.scalar, 'dve': nc.vector, 'pool': nc.gpsimd,
                    'sp': nc.sync, 'gq': nc.gpsimd, 'aq': nc.scalar, 'cc': nc.gpsimd}
        s.sems = []

        def mk(name):
            s.sems.append(es.enter_context(nc.semaphore(name)))
            return len(s.sems) - 1
        s.csem = {e: mk('c_' + e) for e in ('pe', 'act', 'dve', 'pool', 'cc')}
        s.cnt = {e: 0 for e in s.csem}
        s.qsem = {q: [mk('q_%s%d' % (q, i)) for i in range(s.NQ)] for q in ('sp', 'gq', 'aq')}
        s.qn = {q: 0 for q in s.qsem}
        s.waited = {st: {} for st in ('pe', 'act', 'dve', 'pool', 'sp')}
        s.lastw = {}
        s.readers = {}
        s.ninst = 0

    def emit(s, e, fn, reads=(), writes=()):
        st = s.stream[e]
        need = {}

        def add(ev):
            if ev is not None and need.get(ev[0], 0) < ev[1]:
                need[ev[0]] = ev[1]
        for k in reads:
            add(s.lastw.get(k))
        for k in writes:
            add(s.lastw.get(k))
            for sm, v in s.readers.get(k, {}).items():
                add((sm, v))
        if e in s.qsem:
            n = s.qn[e]; slot = n % s.NQ; sem = s.qsem[e][slot]; val = 16 * (n // s.NQ + 1)
            if n >= s.NQ:
                add((sem, val - 16))
            s.qn[e] = n + 1; inc = 16
        else:
            sem = s.csem[e]; s.cnt[e] += 1; val = s.cnt[e]; inc = 1
            if e == 'cc' and val > 1:
                add((sem, val - 1))
        own = s.csem[e] if (e == 'pe' or (e in ('act', 'dve', 'pool') and not SAME_ENGINE_SYNC)) else None
        w = s.waited[st]
        for sm, v in need.items():
            if sm == own or w.get(sm, 0) >= v:
                continue
            s.obj[st].wait_ge(s.sems[sm], v); w[sm] = v
        fn(s.issuer[e]).then_inc(s.sems[sem], inc)
        s.ninst += 1
        for k in reads:
            r = s.readers.setdefault(k, {})
            if r.get(sem, 0) < val:
                r[sem] = val
        for k in writes:
            s.lastw[k] = (sem, val); s.readers[k] = {}

    def fence(s):
        evs = {}
        for e, sem in s.csem.items():
            if s.cnt[e] > 0:
                evs[sem] = s.cnt[e]
        for q, sl in s.qsem.items():
            n = s.qn[q]
            for slot, sem in enumerate(sl):
                uses = max(0, (n - slot + s.NQ - 1) // s.NQ)
                if uses > 0:
                    evs[sem] = 16 * uses
        for st in s.waited:
            own = s.csem.get(st)
            for sm, v in evs.items():
                if sm == own or s.waited[st].get(sm, 0) >= v:
                    continue
                s.obj[st].wait_ge(s.sems[sm], v); s.waited[st][sm] = v
        s.lastw.clear(); s.readers.clear()


def build(cfg, dbg=()):
    nc = bass.Bass("TRN2", target_bir_lowering=False)
    D, KD, TS, TL, CH, LH, CTX, SEQ = cfg.D, cfg.KD, cfg.TS, cfg.TL, cfg.CH, cfg.LH, cfg.CTX, cfg.SEQ
    NROWS, NCH, NCC, MC, MJ = cfg.NROWS, cfg.NCH, cfg.NCC, cfg.MC, cfg.MJ
    FMW, TMW = cfg.FMW, cfg.TMW
    top = ExitStack()
    tr = TR(nc, top)
    E = tr.emit

    def din(name, shape, dt=F32):
        return nc.dram_tensor(name, list(shape), dt, kind="ExternalInput").ap()

    def dscr(name, shape, dt=F32):
        kind = "ExternalOutput" if name in dbg else "Internal"
        return nc.dram_tensor(name, list(shape), dt, kind=kind).ap()

    xT = din("xT", [D, TL])
    c5T = din("c5T", [128, KD, 5])
    wmod = din("wmod", [2, D, MC])
    bmod = din("bmod", [128, 2, MJ])
    normp = din("normp", [128, 5, KD])
    selb = din("selb", [128, 4])
    sele = din("sele", [128, 8])
    wfm = din("wfm", [2, D, FMW])
    wtm = din("wtm", [2, D, TMW])
    ropec = din("ropec", [128, SEQ])
    ropes = din("ropes", [128, SEQ])
    nabi = din("nabi", [2, 128, 2 * 8 * 4 * 64])
    wgp = din("wgp", [2, 2, 32, 128])
    gnorm = din("gnorm", [2, 1, 256])
    smallp = din("smallp", [2, 128, 16])
    lruw = din("lruw", [2, 4, 128, 128])
    wout = din("wout", [2, 5 * 128, D])
    w1d = din("w1d", [D, cfg.FS]); w3d = din("w3d", [D, cfg.FS]); w2d = din("w2d", [cfg.FS, D])
    w1e = din("w1e", [D, cfg.FFE]); w3e = din("w3e", [D, cfg.FFE]); w2e = din("w2e", [cfg.FFE, D])
    rtr = din("rtr", [128, KD, 8])
    trimask = din("trimask", [2, 64, 64])
    chmask = din("chmask", [1, TS])
    identb = din("identb", [128, 128])
    outT = nc.dram_tensor("outT", [D, LH], F32, kind="ExternalOutput").ap()

    modsh = dscr("modsh", [2 * MC, 5]); modall = dscr("modall", [NCORES * 2 * MC, 5])
    xcur = dscr("xcur", [D, TL])
    NS = 2; DH = D // NS; KH = KD // NS
    NUSE = 4
    hsh_u = [[dscr("hsh%d_%d" % (u, i), [DH, TL]) for i in range(NS)] for u in range(NUSE)]
    hfull_u = [[dscr("hfull%d_%d" % (u, i), [NCORES * DH, TL]) for i in range(NS)] for u in range(NUSE)]
    H = {}
    projT = dscr("projT", [4, 9 * 128, TS])
    vcat = dscr("vcat", [4, TS, TMW])
    mixT = dscr("mixT", [4, 5 * 128, TS], BF16)
    ofwd = dscr("ofwd", [4, TS, 256])
    part_u = [[dscr("part%d_%d" % (u, i), [NCORES * DH, TL]) for i in range(NS)] for u in range(NUSE)]
    osh_u = [[dscr("osh%d_%d" % (u, i), [DH, TL]) for i in range(NS)] for u in range(NUSE)]

    def setuse(ag=None, rs=None):
        if ag is not None:
            H['hsh'] = hsh_u[ag]; H['hfull'] = hfull_u[ag]
        if rs is not None:
            H['part'] = part_u[rs]; H['osh'] = osh_u[rs]
    hidmax = max(cfg.FS, cfg.FFE)
    hid = dscr("hid", [NCORES, hidmax, TL], BF16)
    gsh = dscr("gsh", [TL, 8]); gall = dscr("gall", [NCORES * TL, 8]); gmine = dscr("gmine", [1, NCORES * TL])

    uniq = [0]

    def sbt(es, name, shape, dt=F32):
        uniq[0] += 1
        return es.enter_context(nc.sbuf_tensor("%s_%d" % (name, uniq[0]), list(shape), dt))
    ps = top.enter_context(nc.psum_tensor("ps", [128, 8, 512], F32))
    modv = sbt(top, "modv", [128, 2, 2, 6, KD])
    Acoef = sbt(top, "Acoef", [128, 2, 2, 2, KD])
    nrm = sbt(top, "nrm", [128, 5, KD])
    ones32 = sbt(top, "ones32", [128, 128])
    onesbf = sbt(top, "onesbf", [128, 128], BF16)
    idb = sbt(top, "idb", [128, 128], BF16)
    id32 = sbt(top, "id32", [128, 128])
    selb_t = sbt(top, "selb_t", [128, 4]); sele_t = sbt(top, "sele_t", [128, 8])
    E('dve', lambda e: e.memset(ones32[:], 1.0), writes=['ones32'])
    E('dve', lambda e: e.memset(onesbf[:], 1.0), writes=['onesbf'])
    E('sp', lambda e: e.dma_start(out=id32[:], in_=identb), writes=['id32'])
    E('gq', lambda e: e.dma_start(out=idb[:], in_=identb), writes=['idb'])
    E('sp', lambda e: e.dma_start(out=selb_t[:], in_=selb), writes=['selb'])
    E('sp', lambda e: e.dma_start(out=sele_t[:], in_=sele), writes=['sele'])
    E('sp', lambda e: e.dma_start(out=nrm[:], in_=normp), writes=['nrm'])

    PSK = [('ps', i) for i in range(8)]
    psrot = [0]

    def psbank():
        i = psrot[0] % 8; psrot[0] += 1
        return i

    def ltiles(TT):
        out = []
        for s0 in range(0, CH, TT):
            out.append((s0, min(TT, CH - s0), True))
        for s0 in range(CH, TL, TT):
            out.append((s0, min(TT, TL - s0), False))
        return out

    def seqpos(p, s0, isctx):
        return p * CH + s0 if isctx else CTX + p * LH + (s0 - CH)

    def phase_mod():
        with ExitStack() as es:
            c5 = sbt(es, "c5", [128, KD, 5]); sc = sbt(es, "sc", [128, KD, 5])
            bm = sbt(es, "bm", [128, 2, MJ]); mo = sbt(es, "mo", [128, 2, MJ, 5])
            wb = [sbt(es, "wmb%d" % i, [128, KD, 128]) for i in range(2)]
            mall = sbt(es, "mall", [128, 2, NCORES * MJ, 5])
            E('sp', lambda e: e.dma_start(out=c5[:], in_=c5T), writes=['c5'])
            E('sp', lambda e: e.dma_start(out=bm[:], in_=bmod), writes=['bm'])
            E('act', lambda e: e.activation(out=sc[:], in_=c5[:], func=AF.Silu), reads=['c5'], writes=['sc'])
            it = 0
            for l in range(2):
                for j in range(MJ):
                    buf = wb[it % 2]; bk = ('wmb', it % 2); it += 1
                    E('sp', lambda e: e.dma_start(out=buf[:], in_=wmod[l, :, j * 128:(j + 1) * 128].rearrange("(k p) c -> p k c", p=128)), writes=[bk])
                    pb = psbank()
                    for k in range(KD):
                        E('pe', lambda e: e.matmul(ps[:, pb, 0:5], buf[:, k, :], sc[:, k, :], start=(k == 0), stop=(k == KD - 1)),
                          reads=[bk, 'sc'], writes=[PSK[pb]])
                    E('dve', lambda e: e.tensor_scalar(out=mo[:, l, j, :], in0=ps[:, pb, 0:5], scalar1=bm[:, l, j:j + 1], scalar2=None, op0=ALU.add),
                      reads=[PSK[pb], 'bm'], writes=['mo'])
            E('sp', lambda e: e.dma_start(out=modsh.rearrange("(l j p) r -> p l j r", p=128, l=2), in_=mo[:]), reads=['mo'], writes=[])
            tr.fence()
            if NOCC:
                fake_ag(modsh, modall, 2 * MC)
            else:
                E('cc', lambda e: e.collective_compute("AllGather", ALU.bypass, replica_groups=[list(range(NCORES))], ins=[modsh], outs=[modall]),
                  reads=[], writes=['modall'])
            mav = modall.rearrange("(r l j p) x -> l p r j x", p=128, l=2, j=MJ)
            for l in range(2):
                for r in range(NCORES):
                    E('sp', lambda e: e.dma_start(out=mall[:, l, r * MJ:(r + 1) * MJ, :], in_=mav[l, :, r, :, :]), reads=['modall'], writes=['mall'])
            for l in range(2):
                mv = mall[:, l, :, :]
                dst = modv[:, l, 0, :, :].rearrange("p a k -> p (a k)")
                E('dve', lambda e: e.tensor_scalar(out=dst, in0=mv[:, :, 0], scalar1=selb_t[:, 0:1], scalar2=None, op0=ALU.mult),
                  reads=['mall', 'selb'], writes=['modv'])
                for jj in range(1, 4):
                    E('dve', lambda e: e.scalar_tensor_tensor(out=dst, in0=mv[:, :, jj], scalar=selb_t[:, jj:jj + 1], in1=dst, op0=ALU.mult, op1=ALU.add),
                      reads=['mall', 'selb', 'modv'], writes=['modv'])
                dstc = modv[:, l, 1, :, :].rearrange("p a k -> p (a k)")
                E('dve', lambda e: e.tensor_copy(out=dstc, in_=mv[:, :, 4]), reads=['mall'], writes=['modv'])
                for t in range(2):
                    for w, (isc, inr) in enumerate(((1, l), (4, 2 + l))):
                        E('dve', lambda e: e.scalar_tensor_tensor(out=Acoef[:, l, t, w, :], in0=modv[:, l, t, isc, :], scalar=1.0, in1=nrm[:, inr, :], op0=ALU.add, op1=ALU.mult),
                          reads=['modv', 'nrm'], writes=['Acoef'])
            tr.fence()

    def phase_tok(x_in, add_in, G, x_out, A, B, h_out=None, router=False, final=False):
        TT = 128
        if isinstance(add_in, str):
            add_in = H['osh']
        if isinstance(h_out, str):
            h_out = H['hsh']
        with ExitStack() as es:
            xt = [sbt(es, "xt%d" % i, [128, KD, TT]) for i in range(2)]
            ot = [sbt(es, "ot%d" % i, [128, KD, TT]) for i in range(2)] if add_in is not None else None
            hdt = F32
            ht = [sbt(es, "ht%d" % i, [128, KD, TT], hdt) for i in range(2)]
            sqt = sbt(es, "sqt", [128, KD, TT])
            r1 = sbt(es, "r1", [128, TT]); r2 = sbt(es, "r2", [128, TT]); rstd = sbt(es, "rstd", [128, TT])
            if router:
                rt = sbt(es, "rt", [128, KD, 8])
                E('sp', lambda e: e.dma_start(out=rt[:], in_=rtr), writes=['rt'])
                rs_ = {n_: sbt(es, "rs_" + n_, [128, 8]) for n_ in ("lg", "e", "t1", "er", "m2", "t2", "gs", "g")}
                rv_ = {n_: sbt(es, "rv_" + n_, [128, 1]) for n_ in ("m1", "nm1", "e1", "e2", "den", "rden")}
            for ti, (s0, n, isctx) in enumerate(ltiles(TT)):
                if final and isctx:
                    continue
                b2 = ti % 2
                xk, ok, hk, hbk = ('xt', b2), ('ot', b2), ('ht', b2), ('hb', b2)
                x_ = xt[b2]
                E('sp', lambda e: e.dma_start(out=x_[:, :, :n], in_=x_in[:, s0:s0 + n].rearrange("(k p) t -> p k t", p=128)), writes=[xk])
                if add_in is not None:
                    o_ = ot[b2]; g_ = G[1] if isctx else G[0]
                    for hs_ in range(NS):
                        E('sp', lambda e: e.dma_start(out=o_[:, hs_ * KH:(hs_ + 1) * KH, :n], in_=add_in[hs_][:, s0:s0 + n].rearrange("(k p) t -> p k t", p=128)), reads=['osh'], writes=[ok])
                    gbc = g_.unsqueeze(2).broadcast_to([128, KD, n])
                    E('dve', lambda e: e.tensor_tensor(out=o_[:, :, :n], in0=o_[:, :, :n], in1=gbc, op=ALU.mult), reads=[ok], writes=[ok])
                    E('dve', lambda e: e.tensor_tensor(out=x_[:, :, :n], in0=x_[:, :, :n], in1=o_[:, :, :n], op=ALU.add), reads=[ok, xk], writes=[xk])
                if x_out is not None:
                    E('sp', lambda e: e.dma_start(out=x_out[:, s0:s0 + n].rearrange("(k p) t -> p k t", p=128), in_=x_[:, :, :n]), reads=[xk], writes=[])
                pb = psbank()
                E('act', lambda e: e.activation(out=sqt[:, :, :n], in_=x_[:, :, :n], func=AF.Square), reads=[xk], writes=['sqt'])
                for k in range(KD):
                    E('pe', lambda e: e.matmul(ps[:, pb, :n], ones32[:], sqt[:, k, :n], start=(k == 0), stop=(k == KD - 1)),
                      reads=['sqt', 'ones32'], writes=[PSK[pb]])
                E('dve', lambda e: e.tensor_scalar(out=r1[:, :n], in0=ps[:, pb, :n], scalar1=1.0 / D, scalar2=EPS, op0=ALU.mult, op1=ALU.add),
                  reads=[PSK[pb]], writes=['r1'])
                E('act', lambda e: e.activation(out=r2[:, :n], in_=r1[:, :n], func=AF.Sqrt), reads=['r1'], writes=['r2'])
                E('dve', lambda e: e.reciprocal(out=rstd[:, :n], in_=r2[:, :n]), reads=['r2'], writes=['rstd'])
                h_ = ht[b2]
                a_ = A[1] if isctx else A[0]
                b_ = None if B is None else (B[1] if isctx else B[0])
                rbc = rstd[:, :n].unsqueeze(1).broadcast_to([128, KD, n])
                E('dve', lambda e: e.tensor_tensor(out=h_[:, :, :n], in0=x_[:, :, :n], in1=rbc, op=ALU.mult), reads=[xk, 'rstd'], writes=[hk])
                E('dve', lambda e: e.tensor_tensor(out=h_[:, :, :n], in0=h_[:, :, :n], in1=a_.unsqueeze(2).broadcast_to([128, KD, n]), op=ALU.mult), reads=[hk], writes=[hk])
                if b_ is not None:
                    E('pool', lambda e: e.tensor_tensor(out=h_[:, :, :n], in0=h_[:, :, :n], in1=b_.unsqueeze(2).broadcast_to([128, KD, n]), op=ALU.add), reads=[hk], writes=[hk])
                if final:
                    E('sp', lambda e: e.dma_start(out=outT[:, s0 - CH:s0 - CH + n].rearrange("(k p) t -> p k t", p=128), in_=h_[:, :, :n]), reads=[hk], writes=[])
                    continue
                if router:
                    for hs_ in range(NS):
                        E('sp', lambda e: e.dma_start(out=h_out[hs_][:, s0:s0 + n].rearrange("(k p) t -> p k t", p=128), in_=h_[:, hs_ * KH:(hs_ + 1) * KH, :n]), reads=[hk], writes=[])
                    pr = psbank()
                    for k in range(KD):
                        E('pe', lambda e: e.matmul(ps[:n, pr, 0:8], h_[:, k, :n], rt[:, k, :], start=(k == 0), stop=(k == KD - 1)),
                          reads=[hk, 'rt'], writes=[PSK[pr]])
                    R, V = rs_, rv_
                    rk = ['rtmp']
                    E('dve', lambda e: e.tensor_copy(out=R['lg'][:n], in_=ps[:n, pr, 0:8]), reads=[PSK[pr]], writes=rk)
                    E('dve', lambda e: e.reduce_max(out=V['m1'][:n], in_=R['lg'][:n], axis=AX.X), reads=rk, writes=rk)
                    E('dve', lambda e: e.tensor_scalar(out=V['nm1'][:n], in0=V['m1'][:n], scalar1=-1.0, scalar2=None, op0=ALU.mult), reads=rk, writes=rk)
                    E('act', lambda e: e.activation(out=R['e'][:n], in_=R['lg'][:n], func=AF.Exp, bias=V['nm1'][:n, 0:1]), reads=rk, writes=rk)
                    E('dve', lambda e: e.reduce_max(out=V['e1'][:n], in_=R['e'][:n], axis=AX.X), reads=rk, writes=rk)
                    E('dve', lambda e: e.scalar_tensor_tensor(out=R['t1'][:n], in0=R['e'][:n], scalar=V['e1'][:n, 0:1], in1=R['e'][:n], op0=ALU.is_equal, op1=ALU.mult), reads=rk, writes=rk)
                    E('dve', lambda e: e.tensor_tensor(out=R['er'][:n], in0=R['e'][:n], in1=R['t1'][:n], op=ALU.subtract), reads=rk, writes=rk)
                    E('dve', lambda e: e.reduce_max(out=V['e2'][:n], in_=R['er'][:n], axis=AX.X), reads=rk, writes=rk)
                    E('dve', lambda e: e.scalar_tensor_tensor(out=R['t2'][:n], in0=R['er'][:n], scalar=V['e2'][:n, 0:1], in1=R['er'][:n], op0=ALU.is_equal, op1=ALU.mult), reads=rk, writes=rk)
                    E('dve', lambda e: e.tensor_tensor(out=R['gs'][:n], in0=R['t1'][:n], in1=R['t2'][:n], op=ALU.add), reads=rk, writes=rk)
                    E('dve', lambda e: e.tensor_tensor(out=V['den'][:n], in0=V['e1'][:n], in1=V['e2'][:n], op=ALU.add), reads=rk, writes=rk)
                    E('dve', lambda e: e.reciprocal(out=V['rden'][:n], in_=V['den'][:n]), reads=rk, writes=rk)
                    E('dve', lambda e: e.tensor_scalar(out=R['g'][:n], in0=R['gs'][:n], scalar1=V['rden'][:n, 0:1], scalar2=None, op0=ALU.mult), reads=rk, writes=rk)
                    E('sp', lambda e: e.dma_start(out=gsh[s0:s0 + n, :], in_=R['g'][:n]), reads=rk, writes=[])
                else:
                    for hs_ in range(NS):
                        E('sp', lambda e: e.dma_start(out=h_out[hs_][:, s0:s0 + n].rearrange("(k p) t -> p k t", p=128), in_=h_[:, hs_ * KH:(hs_ + 1) * KH, :n]), reads=[hk], writes=[])
            tr.fence()

    import os as _os2
    NOCC = _os2.environ.get("NOCC", "0") == "1"

    def fake_ag(src, dst, rows):
        for r in range(NCORES):
            E('gq', lambda e: e.dma_start(out=dst[r * rows:(r + 1) * rows, :], in_=src), reads=[], writes=['hfull', 'modall', 'gall'])

    def allgather_h():
        if NOCC:
            for i in range(NS):
                fake_ag(H['hsh'][i], H['hfull'][i], DH)
            return
        for i in range(NS):
            E('cc', lambda e: e.collective_compute("AllGather", ALU.bypass, replica_groups=[list(range(NCORES))], ins=[H['hsh'][i]], outs=[H['hfull'][i]]),
              reads=[], writes=['hfull'])

    def reduce_scatter(halves=None):
        halves = list(range(NS)) if halves is None else halves
        if NOCC:
            for i in halves:
                E('gq', lambda e: e.dma_start(out=H['osh'][i], in_=H['part'][i][0:DH, :]), reads=[], writes=['osh', ('part', i)])
            return
        for i in halves:
            E('cc', lambda e: e.collective_compute("ReduceScatter", ALU.add, replica_groups=[list(range(NCORES))], ins=[H['part'][i]], outs=[H['osh'][i]]),
              reads=[], writes=['osh', ('part', i)])

    QS = 128 ** -0.5
    SLOT = {0: 0, 1: 1, 2: 2, 3: 3, 4: 4, 5: 5, 8: 6, 9: 7, 10: 8}

    def phase_proj(l):
        TT = 256
        with ExitStack() as es:
            W = sbt(es, "Wfm", [128, KD, FMW], BF16)
            for k0 in range(0, KD, 4):
                k1 = min(KD, k0 + 4)
                E('gq', lambda e: e.dma_start(out=W[:, k0:k1, :], in_=wfm[l, k0 * 128:k1 * 128, :].rearrange("(k p) c -> p k c", p=128)), writes=['W'])
            hT = [sbt(es, "hT%d" % i, [128, KD, TT], BF16) for i in range(2)]
            st = [sbt(es, "fst%d" % i, [128, 9, TT]) for i in range(2)]
            rc = [sbt(es, "rc%d" % i, [128, TT]) for i in range(2)]
            rsn = [sbt(es, "rsn%d" % i, [128, TT]) for i in range(2)]
            t1 = sbt(es, "rt1", [128, TT]); t2 = sbt(es, "rt2", [128, TT])
            for i in range(2):
                E('dve', lambda e: e.memset(st[i][:], 0.0), writes=[('fst', i)])
            it = 0
            for r in range(NCORES):
                b, p = r // 2, r % 2
                for (s0, n, isctx) in ltiles(TT):
                    b2 = it % 2; it += 1
                    hk, sk = ('hT', b2), ('fst', b2)
                    h_ = hT[b2]; s_ = st[b2]
                    pos = seqpos(p, s0, isctx)
                    for hs_ in range(NS):
                        E('gq', lambda e: e.dma_start(out=h_[:, hs_ * KH:(hs_ + 1) * KH, :n], in_=H['hfull'][hs_][r * DH:(r + 1) * DH, s0:s0 + n].rearrange("(k p) t -> p k t", p=128)), reads=['hfull'], writes=[hk])
                    if not isctx:
                        lp = pos - CTX
                        E('sp', lambda e: e.dma_start(out=rc[b2][:, :n], in_=ropec[:, lp:lp + n]), writes=[('rc', b2)])
                        E('sp', lambda e: e.dma_start(out=rsn[b2][:, :n], in_=ropes[:, lp:lp + n]), writes=[('rsn', b2)])
                    banks = {}
                    for g in range(11):
                        c0 = g * 128; M = 32 if g == 8 else 128
                        pb = psbank(); banks[g] = pb
                        for k in range(KD):
                            E('pe', lambda e: e.matmul(ps[:M, pb, :n], W[:, k, c0:c0 + M], h_[:, k, :n], start=(k == 0), stop=(k == KD - 1)),
                              reads=['W', hk], writes=[PSK[pb]])
                        if g in (6, 7) and isctx:
                            continue
                        if g in (4, 5):
                            continue
                        if g in (6, 7):
                            g0 = g - 2; pq = banks[g0]; sc_ = QS if g0 == 4 else 1.0
                            E('dve', lambda e: e.tensor_tensor(out=t1[:, :n], in0=ps[:, pq, :n], in1=rc[b2][:, :n], op=ALU.mult), reads=[PSK[pq], ('rc', b2)], writes=['rt1'])
                            E('dve', lambda e: e.tensor_tensor(out=t2[:, :n], in0=ps[:, pb, :n], in1=rsn[b2][:, :n], op=ALU.mult), reads=[PSK[pb], ('rsn', b2)], writes=['rt2'])
                            E('dve', lambda e: e.tensor_tensor(out=t1[:, :n], in0=t1[:, :n], in1=t2[:, :n], op=ALU.add), reads=['rt1', 'rt2'], writes=['rt1'])
                            E('act', lambda e: e.activation(out=s_[:, SLOT[g0], :n], in_=t1[:, :n], func=AF.Copy, scale=sc_), reads=['rt1'], writes=[sk])
                            continue
                        sc_ = QS if g in (0, 1) else 1.0
                        if g % 2 == 0:
                            E('act', lambda e: e.activation(out=s_[:M, SLOT[g], :n], in_=ps[:M, pb, :n], func=AF.Copy, scale=sc_), reads=[PSK[pb]], writes=[sk])
                        else:
                            E('dve', lambda e: e.tensor_scalar(out=s_[:M, SLOT[g], :n], in0=ps[:M, pb, :n], scalar1=sc_, scalar2=None, op0=ALU.mult), reads=[PSK[pb]], writes=[sk])
                    if isctx:
                        for g0 in (4, 5):
                            pq = banks[g0]; sc_ = QS if g0 == 4 else 1.0
                            E('act', lambda e: e.activation(out=s_[:, SLOT[g0], :n], in_=ps[:, pq, :n], func=AF.Copy, scale=sc_), reads=[PSK[pq]], writes=[sk])
                    E('sp', lambda e: e.dma_start(out=projT[b].rearrange("(g p) t -> p g t", p=128)[:, :, pos:pos + n], in_=s_[:, :, :n]), reads=[sk], writes=[])
            tr.fence()
        with ExitStack() as es:
            W = sbt(es, "Wtm", [128, KD, TMW], BF16)
            for k0 in range(0, KD, 4):
                k1 = min(KD, k0 + 4)
                E('gq', lambda e: e.dma_start(out=W[:, k0:k1, :], in_=wtm[l, k0 * 128:k1 * 128, :].rearrange("(k p) c -> p k c", p=128)), writes=['W'])
            hT = [sbt(es, "hT%d" % i, [128, KD, TT], BF16) for i in range(2)]
            st = [sbt(es, "tst%d" % i, [128, TMW]) for i in range(2)]
            it = 0; si = 0
            for r in range(NCORES):
                b, p = r // 2, r % 2
                for (s0, n, isctx) in ltiles(TT):
                    b2 = it % 2; it += 1
                    hk = ('hT', b2); h_ = hT[b2]
                    pos = seqpos(p, s0, isctx)
                    for hs_ in range(NS):
                        E('gq', lambda e: e.dma_start(out=h_[:, hs_ * KH:(hs_ + 1) * KH, :n], in_=H['hfull'][hs_][r * DH:(r + 1) * DH, s0:s0 + n].rearrange("(k p) t -> p k t", p=128)), reads=['hfull'], writes=[hk])
                    for u0 in range(0, n, 128):
                        m = min(128, n - u0)
                        s2 = si % 2; si += 1
                        sk = ('tst', s2); s_ = st[s2]
                        pa = psbank(); pbk = psbank()
                        for k in range(KD):
                            E('pe', lambda e: e.matmul(ps[:m, pa, 0:512], h_[:, k, u0:u0 + m], W[:, k, 0:512], start=(k == 0), stop=(k == KD - 1)),
                              reads=['W', hk], writes=[PSK[pa]])
                        for k in range(KD):
                            E('pe', lambda e: e.matmul(ps[:m, pbk, 0:256], h_[:, k, u0:u0 + m], W[:, k, 512:768], start=(k == 0), stop=(k == KD - 1)),
                              reads=['W', hk], writes=[PSK[pbk]])
                        E('act', lambda e: e.activation(out=s_[:m, 0:512], in_=ps[:m, pa, 0:512], func=AF.Copy), reads=[PSK[pa]], writes=[sk])
                        E('dve', lambda e: e.tensor_copy(out=s_[:m, 512:768], in_=ps[:m, pbk, 0:256]), reads=[PSK[pbk]], writes=[sk])
                        E('sp', lambda e: e.dma_start(out=vcat[b, pos + u0:pos + u0 + m, :], in_=s_[:m, :]), reads=[sk], writes=[])
            tr.fence()

    def phase_na(l, with_ctx):
        NT = TS // 128
        NKC = CTX // 128
        with ExitStack() as es:
            nab = sbt(es, "nab", [128, 2, 8, 4, 64])
            E('sp', lambda e: e.dma_start(out=nab[:].rearrange("p a b c d -> p (a b c d)"), in_=nabi[l]), writes=['nab'])
            qT = [sbt(es, "naq%d" % i, [128, TS], BF16) for i in range(2)]
            kT = [sbt(es, "nak%d" % i, [128, TS], BF16) for i in range(2)]
            vev = [sbt(es, "vev%d" % i, [128, NT, 128], BF16) for i in range(2)]
            vod = [sbt(es, "vod%d" % i, [128, NT - 1, 128], BF16) for i in range(2)]
            mx = [sbt(es, "mx%d" % i, [128, TS], BF16) for i in range(2)]
            tmpb = [sbt(es, "ntmp%d" % i, [128, 4, 64]) for i in range(2)]
            pT = [sbt(es, "npT%d" % i, [128, NKC + 4, 64], BF16) for i in range(2)]
            pTc = sbt(es, "npTc", [128, NKC, CTX], BF16)
            rsm = [sbt(es, "nrs%d" % i, [128, 256]) for i in range(2)]
            it = 0; ri = 0
            for b in range(4):
                for j in range(2):
                    b2 = it % 2; it += 1
                    q_, k_, ve_, vo_, m_ = qT[b2], kT[b2], vev[b2], vod[b2], mx[b2]
                    qk, kk, vek, vok, mk_ = ('naq', b2), ('nak', b2), ('vev', b2), ('vod', b2), ('mx', b2)
                    E('gq', lambda e: e.dma_start(out=q_[:], in_=projT[b, j * 128:(j + 1) * 128, :]), reads=[], writes=[qk])
                    E('gq', lambda e: e.dma_start(out=k_[:], in_=projT[b, (2 + j) * 128:(3 + j) * 128, :]), reads=[], writes=[kk])
                    E('gq', lambda e: e.dma_start(out=ve_[:], in_=vcat[b, :, j * 128:(j + 1) * 128].rearrange("(m p) d -> p m d", p=128)), reads=[], writes=[vek])
                    E('gq', lambda e: e.dma_start(out=vo_[:], in_=vcat[b, 64:TS - 64, j * 128:(j + 1) * 128].rearrange("(m p) d -> p m d", p=128)), reads=[], writes=[vok])
                    if with_ctx:
                        pS = psbank(); pO = psbank()
                        for c in range(NKC):
                            E('pe', lambda e: e.matmul(ps[:, pS, c * CTX:(c + 1) * CTX], k_[:, c * 128:(c + 1) * 128], q_[:, 0:CTX], start=True, stop=True),
                              reads=[qk, kk], writes=[PSK[pS]])
                        E('act', lambda e: e.activation(out=pTc[:].rearrange("p c q -> p (c q)"), in_=ps[:, pS, 0:NKC * CTX], func=AF.Exp), reads=[PSK[pS]], writes=['pTc'])
                        for c in range(NKC):
                            E('pe', lambda e: e.matmul(ps[:, pO, 0:CTX], ve_[:, c, :], pTc[:, c, :], start=(c == 0), stop=(c == NKC - 1)), reads=[vek, 'pTc'], writes=[PSK[pO]])
                        for c in range(NKC):
                            E('pe', lambda e: e.matmul(ps[:, pO, 256:256 + CTX], onesbf[:], pTc[:, c, :], start=(c == 0), stop=(c == NKC - 1)), reads=['onesbf', 'pTc'], writes=[PSK[pO]])
                        r2 = ri % 2; ri += 1
                        E('dve', lambda e: e.reciprocal(out=rsm[r2][:, :CTX], in_=ps[:, pO, 256:256 + CTX]), reads=[PSK[pO]], writes=[('nrs', r2)])
                        E('dve', lambda e: e.tensor_tensor(out=m_[:, 0:CTX], in0=ps[:, pO, 0:CTX], in1=rsm[r2][:, :CTX], op=ALU.mult), reads=[PSK[pO], ('nrs', r2)], writes=[mk_])
                    else:
                        E('dve', lambda e: e.memset(m_[:, 0:CTX], 0.0), writes=[mk_])
                    for r in range(NROWS):
                        rs0 = min(max(r - 4, 0), NROWS - 8)
                        cfgi = r - rs0
                        qs = CTX + 64 * r
                        pS = psbank(); pO = psbank()
                        r2 = ri % 2; ri += 1
                        tk, pk, rk = ('ntmp', r2), ('npT', r2), ('nrs', r2)
                        vt = []
                        for c in range(NKC):
                            E('pe', lambda e: e.matmul(ps[:, pS, c * 64:(c + 1) * 64], k_[:, c * 128:(c + 1) * 128], q_[:, qs:qs + 64], start=True, stop=True),
                              reads=[qk, kk], writes=[PSK[pS]])
                            vt.append((ve_, c, vek))
                        for jj in range(4):
                            row0 = rs0 + 2 * jj
                            ks = CTX + 64 * row0
                            c = NKC + jj
                            E('pe', lambda e: e.matmul(ps[:, pS, c * 64:(c + 1) * 64], k_[:, ks:ks + 128], q_[:, qs:qs + 64], start=True, stop=True),
                              reads=[qk, kk], writes=[PSK[pS]])
                            if row0 % 2 == 0:
                                vt.append((ve_, NKC + row0 // 2, vek))
                            else:
                                vt.append((vo_, (ks - 64) // 128, vok))
                        E('dve', lambda e: e.tensor_tensor(out=tmpb[r2][:], in0=ps[:, pS, NKC * 64:(NKC + 4) * 64].rearrange("p (c q) -> p c q", q=64),
                                                           in1=nab[:, j, cfgi, :, :], op=ALU.add), reads=[PSK[pS], 'nab'], writes=[tk])
                        E('act', lambda e: e.activation(out=pT[r2][:, NKC:, :], in_=tmpb[r2][:], func=AF.Exp), reads=[tk], writes=[pk])
                        E('act', lambda e: e.activation(out=pT[r2][:, 0:NKC, :], in_=ps[:, pS, 0:NKC * 64].rearrange("p (c q) -> p c q", q=64), func=AF.Exp), reads=[PSK[pS]], writes=[pk])
                        nk = NKC + 4
                        for c in range(nk):
                            vtile, vi, vkey = vt[c]
                            E('pe', lambda e: e.matmul(ps[:, pO, 0:64], vtile[:, vi, :], pT[r2][:, c, :], start=(c == 0), stop=(c == nk - 1)), reads=[vkey, pk], writes=[PSK[pO]])
                        for c in range(nk):
                            E('pe', lambda e: e.matmul(ps[:, pO, 64:128], onesbf[:], pT[r2][:, c, :], start=(c == 0), stop=(c == nk - 1)), reads=['onesbf', pk], writes=[PSK[pO]])
                        E('dve', lambda e: e.reciprocal(out=rsm[r2][:, :64], in_=ps[:, pO, 64:128]), reads=[PSK[pO]], writes=[rk])
                        E('dve', lambda e: e.tensor_tensor(out=m_[:, qs:qs + 64], in0=ps[:, pO, 0:64], in1=rsm[r2][:, :64], op=ALU.mult), reads=[PSK[pO], rk], writes=[mk_])
                    E('sp', lambda e: e.dma_start(out=mixT[b, j * 128:(j + 1) * 128, :], in_=m_[:]), reads=[mk_], writes=[])
            tr.fence()

    def phase_gla(l, with_ctx):
        with ExitStack() as es:
            Q = sbt(es, "gQ", [128, TS]); K = sbt(es, "gK", [128, TS])
            X1 = sbt(es, "gX1", [128, TS]); X2 = sbt(es, "gX2", [128, TS]); X3 = sbt(es, "gX3", [128, TS])
            LR = sbt(es, "gLR", [32, TS])
            qd = sbt(es, "gqd", [128, TS], BF16); ki = sbt(es, "gki", [128, TS], BF16); ke = sbt(es, "gke", [128, TS], BF16)
            V = sbt(es, "gV", [64, NCH, 256], BF16)
            cm = sbt(es, "gcm", [128, TS], BF16)
            tri = sbt(es, "gtri", [64, 2, 64])
            wg = sbt(es, "gwg", [32, 2, 128])
            sp_ = sbt(es, "gsp", [128, 16])
            nbg = sbt(es, "gnbg", [128, 2])
            gn = sbt(es, "ggn", [64, 256])
            bend = sbt(es, "gbend", [128, NCH]); dec = sbt(es, "gdec", [128, NCH])
            S = sbt(es, "gS", [128, 256]); Sb = sbt(es, "gSb", [128, 256], BF16)
            att = [sbt(es, "gatt%d" % i, [64, 64], BF16) for i in range(2)]
            kes = [sbt(es, "gkes%d" % i, [64, 128], BF16) for i in range(2)]
            of_ = [sbt(es, "gof%d" % i, [64, 256]) for i in range(2)]
            gch = [sbt(es, "ggc%d" % i, [64, 256]) for i in range(2)]
            osum = [sbt(es, "gos%d" % i, [64, 256]) for i in range(2)]
            junk = sbt(es, "gjunk", [64, 256])
            y1 = [sbt(es, "gy%d" % i, [64, 256]) for i in range(2)]
            sg = [sbt(es, "gsg%d" % i, [64, 256]) for i in range(2)]
            ssq = [sbt(es, "gssq%d" % i, [64, 4]) for i in range(2)]
            mo_ = [sbt(es, "gmo%d" % i, [128, 2, 64], BF16) for i in range(2)]
            E('gq', lambda e: e.dma_start(out=cm[:], in_=chmask.partition_broadcast(128)), writes=['cm'])
            E('sp', lambda e: e.dma_start(out=tri[:], in_=trimask.rearrange("d j i -> j d i")), writes=['tri'])
            E('sp', lambda e: e.dma_start(out=wg[:], in_=wgp[l].rearrange("d r c -> r d c")), writes=['wg'])
            E('sp', lambda e: e.dma_start(out=sp_[:], in_=smallp[l]), writes=['sp_'])
            E('sp', lambda e: e.dma_start(out=gn[:], in_=gnorm[l].partition_broadcast(64)), writes=['gn'])
            E('dve', lambda e: e.tensor_scalar(out=nbg[:], in0=sp_[:, 11:13], scalar1=-1.0, scalar2=None, op0=ALU.mult), reads=['sp_'], writes=['nbg'])
            if not with_ctx:
                zt = sbt(es, "gzt", [128, 2, CTX], BF16)
                E('dve', lambda e: e.memset(zt[:], 0.0), writes=['zt'])
            ci = 0
            for b in range(4):
                E('sp', lambda e: e.dma_start(out=Q[:], in_=projT[b, 4 * 128:5 * 128, :]), reads=[], writes=['Q'])
                E('sp', lambda e: e.dma_start(out=K[:], in_=projT[b, 5 * 128:6 * 128, :]), reads=[], writes=['K'])
                E('sp', lambda e: e.dma_start(out=LR[:], in_=projT[b, 6 * 128:6 * 128 + 32, :]), reads=[], writes=['LR'])
                E('gq', lambda e: e.dma_start(out=V[:], in_=vcat[b, :, 256:512].rearrange("(c p) d -> p c d", p=64)), reads=[], writes=['V'])
                if not with_ctx:
                    E('sp', lambda e: e.dma_start(out=mixT[b, 2 * 128:4 * 128, 0:CTX].rearrange("(s p) t -> p s t", p=128), in_=zt[:]), reads=['zt'], writes=[])
                for d in range(2):
                    for t0 in range(0, TS, 512):
                        n = min(512, TS - t0)
                        pb = psbank()
                        E('pe', lambda e: e.matmul(ps[:, pb, :n], wg[:, d, :], LR[:, t0:t0 + n], start=True, stop=True), reads=['wg', 'LR'], writes=[PSK[pb]])
                        E('act', lambda e: e.activation(out=X2[:, t0:t0 + n], in_=ps[:, pb, :n], func=AF.Exp, scale=-1.0, bias=nbg[:, d:d + 1]), reads=[PSK[pb], 'nbg'], writes=['X2'])
                    E('act', lambda e: e.activation(out=X1[:], in_=X2[:], func=AF.Ln, bias=1.0), reads=['X2'], writes=['X1'])
                    E('dve', lambda e: e.tensor_scalar(out=X1[:], in0=X1[:], scalar1=-1.0 / 16.0, scalar2=None, op0=ALU.mult), reads=['X1'], writes=['X1'])
                    E('dve', lambda e: e.tensor_tensor_scan(out=X2[:], data0=cm[:], data1=X1[:], initial=0.0, op0=ALU.mult, op1=ALU.add), reads=['cm', 'X1', 'X2'], writes=['X2'])
                    X2v = X2[:].rearrange("p (c j) -> p c j", j=64)
                    X3v = X3[:].rearrange("p (c j) -> p c j", j=64)
                    E('dve', lambda e: e.tensor_copy(out=bend[:], in_=X2v[:, :, 63]), reads=['X2'], writes=['bend'])
                    E('act', lambda e: e.activation(out=dec[:], in_=bend[:], func=AF.Exp), reads=['bend'], writes=['dec'])
                    E('dve', lambda e: e.tensor_tensor(out=X3v, in0=X2v[:, :, 63:64].broadcast_to([128, NCH, 64]), in1=X2v, op=ALU.subtract), reads=['X2', 'X3'], writes=['X3'])
                    if d == 0:
                        E('act', lambda e: e.activation(out=X3[:], in_=X3[:], func=AF.Exp), reads=['X3'], writes=['X3'])
                        E('dve', lambda e: e.tensor_tensor(out=ke[:], in0=K[:], in1=X3[:], op=ALU.mult), reads=['K', 'X3', 'ke'], writes=['ke'])
                        E('act', lambda e: e.activation(out=X3[:], in_=X2[:], func=AF.Exp), reads=['X2', 'X3', 'ke'], writes=['X3'])
                        E('dve', lambda e: e.tensor_tensor(out=qd[:], in0=Q[:], in1=X3[:], op=ALU.mult), reads=['Q', 'X3', 'qd'], writes=['qd'])
                        E('act', lambda e: e.activation(out=X3[:], in_=X2[:], func=AF.Exp, scale=-1.0), reads=['X2', 'X3', 'qd'], writes=['X3'])
                        E('dve', lambda e: e.tensor_tensor(out=ki[:], in0=K[:], in1=X3[:], op=ALU.mult), reads=['K', 'X3', 'ki'], writes=['ki'])
                    else:
                        E('dve', lambda e: e.tensor_tensor(out=X3[:], in0=X3[:], in1=X1[:], op=ALU.add), reads=['X3', 'X1'], writes=['X3'])
                        E('dve', lambda e: e.tensor_tensor(out=X1[:], in0=X2[:], in1=X1[:], op=ALU.subtract), reads=['X2', 'X1'], writes=['X1'])
                        E('act', lambda e: e.activation(out=X1[:], in_=X1[:], func=AF.Exp), reads=['X1'], writes=['X1'])
                        E('dve', lambda e: e.tensor_tensor(out=ke[:], in0=K[:], in1=X1[:], op=ALU.mult), reads=['K', 'X1', 'ke'], writes=['ke'])
                        E('act', lambda e: e.activation(out=X2[:], in_=X3[:], func=AF.Exp), reads=['X3', 'X2'], writes=['X2'])
                        E('dve', lambda e: e.tensor_tensor(out=qd[:], in0=Q[:], in1=X2[:], op=ALU.mult), reads=['Q', 'X2', 'qd'], writes=['qd'])
                        E('act', lambda e: e.activation(out=X2[:], in_=X3[:], func=AF.Exp, scale=-1.0), reads=['X3', 'X2', 'qd'], writes=['X2'])
                        E('dve', lambda e: e.tensor_tensor(out=ki[:], in0=K[:], in1=X2[:], op=ALU.mult), reads=['K', 'X2', 'ki'], writes=['ki'])
                    E('dve', lambda e: e.memset(S[:], 0.0), reads=['S'], writes=['S'])
                    E('dve', lambda e: e.memset(Sb[:], 0.0), reads=['Sb'], writes=['Sb'])
                    if d == 0:
                        order = list(range(NCH))
                    else:
                        order = list(range(NCC - 1, -1, -1)) + list(range(NCH - 1, NCC - 1, -1))
                    for c in order:
                        t0 = 64 * c
                        isctx = c < NCC
                        need_o = with_ctx or not isctx
                        c2 = ci % 2; ci += 1
                        ak, kek = ('gatt', c2), ('gkes', c2)
                        pT_ = psbank()
                        pst = ps[:, pT_, :].bitcast(BF16)
                        E('pe', lambda e: e.transpose(pst[0:64, 0:128], ke[:, t0:t0 + 64], idb[:]), reads=['ke', 'idb'], writes=[PSK[pT_]])
                        E('act', lambda e: e.activation(out=kes[c2][:], in_=pst[0:64, 0:128], func=AF.Copy), reads=[PSK[pT_]], writes=[kek])
                        if need_o:
                            pA = psbank(); pO = psbank()
                            E('pe', lambda e: e.matmul(ps[0:64, pA, 0:64], ki[:, t0:t0 + 64], qd[:, t0:t0 + 64], start=True, stop=True), reads=['ki', 'qd'], writes=[PSK[pA]])
                            E('dve', lambda e: e.tensor_tensor(out=att[c2][:], in0=ps[0:64, pA, 0:64], in1=tri[:, d, :], op=ALU.mult), reads=[PSK[pA], 'tri'], writes=[ak])
                            E('pe', lambda e: e.matmul(ps[0:64, pO, 0:256], att[c2][:], V[:, c, :], start=True, stop=False), reads=[ak, 'V'], writes=[PSK[pO]])
                            E('pe', lambda e: e.matmul(ps[0:64, pO, 0:256], qd[:, t0:t0 + 64], Sb[:], start=False, stop=True), reads=['qd', 'Sb'], writes=[PSK[pO]])
                        pD = psbank()
                        E('pe', lambda e: e.matmul(ps[:, pD, 0:256], kes[c2][:], V[:, c, :], start=True, stop=True), reads=[kek, 'V'], writes=[PSK[pD]])
                        E('dve', lambda e: e.scalar_tensor_tensor(out=S[:], in0=S[:], scalar=dec[:, c:c + 1], in1=ps[:, pD, 0:256], op0=ALU.mult, op1=ALU.add),
                          reads=['S', 'dec', PSK[pD]], writes=['S'])
                        E('act', lambda e: e.activation(out=Sb[:], in_=S[:], func=AF.Copy), reads=['S'], writes=['Sb'])
                        if not need_o:
                            continue
                        if d == 0:
                            E('act', lambda e: e.activation(out=of_[c2][:], in_=ps[0:64, pO, 0:256], func=AF.Copy), reads=[PSK[pO]], writes=[('gof', c2)])
                            E('sp', lambda e: e.dma_start(out=ofwd[b, t0:t0 + 64, :], in_=of_[c2][:]), reads=[('gof', c2)], writes=[('ofwd', c)])
                        else:
                            fk, gk, ok_, yk, sk, qk_, mk_ = ('gof', c2), ('ggc', c2), ('gos', c2), ('gy', c2), ('gsg', c2), ('gssq', c2), ('gmo', c2)
                            E('sp', lambda e: e.dma_start(out=of_[c2][:], in_=ofwd[b, t0:t0 + 64, :]), reads=[('ofwd', c)], writes=[fk])
                            E('sp', lambda e: e.dma_start(out=gch[c2][:], in_=vcat[b, t0:t0 + 64, 512:768]), reads=[], writes=[gk])
                            E('dve', lambda e: e.tensor_tensor(out=osum[c2][:], in0=ps[0:64, pO, 0:256], in1=of_[c2][:], op=ALU.add), reads=[PSK[pO], fk], writes=[ok_])
                            E('act', lambda e: e.activation(out=junk[:], in_=osum[c2][:], func=AF.Square, accum_out=ssq[c2][:, 0:1]), reads=[ok_], writes=['junk', qk_])
                            E('dve', lambda e: e.tensor_scalar(out=ssq[c2][:, 1:2], in0=ssq[c2][:, 0:1], scalar1=1.0 / 256.0, scalar2=EPS, op0=ALU.mult, op1=ALU.add), reads=[qk_], writes=[qk_])
                            E('act', lambda e: e.activation(out=ssq[c2][:, 2:3], in_=ssq[c2][:, 1:2], func=AF.Sqrt), reads=[qk_], writes=[qk_])
                            E('dve', lambda e: e.reciprocal(out=ssq[c2][:, 3:4], in_=ssq[c2][:, 2:3]), reads=[qk_], writes=[qk_])
                            E('act', lambda e: e.activation(out=sg[c2][:], in_=gch[c2][:], func=AF.Silu), reads=[gk], writes=[sk])
                            E('dve', lambda e: e.scalar_tensor_tensor(out=y1[c2][:], in0=osum[c2][:], scalar=ssq[c2][:, 3:4], in1=gn[:], op0=ALU.mult, op1=ALU.mult), reads=[ok_, qk_, 'gn'], writes=[yk])
                            E('dve', lambda e: e.tensor_tensor(out=y1[c2][:], in0=y1[c2][:], in1=sg[c2][:], op=ALU.mult), reads=[yk, sk], writes=[yk])
                            pX = psbank()
                            for hh in range(2):
                                E('pe', lambda e: e.transpose(ps[:, pX, hh * 64:(hh + 1) * 64], y1[c2][:, hh * 128:(hh + 1) * 128], id32[0:64, 0:64]), reads=[yk, 'id32'], writes=[PSK[pX]])
                            E('act', lambda e: e.activation(out=mo_[c2][:].rearrange("p s t -> p (s t)"), in_=ps[:, pX, 0:128], func=AF.Copy), reads=[PSK[pX]], writes=[mk_])
                            E('sp', lambda e: e.dma_start(out=mixT[b, 2 * 128:4 * 128, t0:t0 + 64].rearrange("(s p) t -> p s t", p=128), in_=mo_[c2][:]), reads=[mk_], writes=[])
            tr.fence()

    def phase_lru(l):
        with ExitStack() as es:
            RX = sbt(es, "lRX", [128, TS]); RG = sbt(es, "lRG", [128, TS]); XC = sbt(es, "lXC", [128, TS])
            A_ = sbt(es, "lA", [128, TS]); B_ = sbt(es, "lB", [128, TS]); T1 = sbt(es, "lT1", [128, TS])
            HF = sbt(es, "lHF", [128, TS]); HB = sbt(es, "lHB", [128, TS])
            MX = sbt(es, "lMX", [128, TS], BF16)
            sp_ = sbt(es, "lsp", [128, 16]); sl = sbt(es, "lsl", [128, 4])
            lw = sbt(es, "llw", [128, 4, 128])
            hb0 = sbt(es, "lhb0", [128, 1])
            E('sp', lambda e: e.dma_start(out=sp_[:], in_=smallp[l]), writes=['sp_'])
            E('sp', lambda e: e.dma_start(out=lw[:], in_=lruw[l].rearrange("g c d -> c g d")), writes=['lw'])
            E('act', lambda e: e.activation(out=sl[:, 0:2], in_=sp_[:, 9:11], func=AF.Exp, scale=-1.0), reads=['sp_'], writes=['sl'])
            E('act', lambda e: e.activation(out=sl[:, 0:2], in_=sl[:, 0:2], func=AF.Ln, bias=1.0), reads=['sl'], writes=['sl'])
            E('dve', lambda e: e.tensor_scalar(out=sl[:, 2:4], in0=sl[:, 0:2], scalar1=-8.0, scalar2=None, op0=ALU.mult), reads=['sl'], writes=['sl'])
            segs = [(0, CTX), (CTX, TS)]
            for b in range(4):
                E('sp', lambda e: e.dma_start(out=RX[:], in_=projT[b, 7 * 128:8 * 128, :]), reads=[], writes=['RX'])
                E('sp', lambda e: e.dma_start(out=RG[:], in_=projT[b, 8 * 128:9 * 128, :]), reads=[], writes=['RG'])
                E('dve', lambda e: e.tensor_scalar(out=XC[:], in0=RX[:], scalar1=sp_[:, 2:3], scalar2=sp_[:, 4:5], op0=ALU.mult, op1=ALU.add), reads=['RX', 'sp_', 'XC'], writes=['XC'])
                for (a0, a1) in segs:
                    for jt, off in ((0, -2), (1, -1), (3, 1)):
                        if off < 0:
                            o0, o1, i0, i1 = a0 - off, a1, a0, a1 + off
                        else:
                            o0, o1, i0, i1 = a0, a1 - off, a0 + off, a1
                        E('dve', lambda e: e.scalar_tensor_tensor(out=XC[:, o0:o1], in0=RX[:, i0:i1], scalar=sp_[:, jt:jt + 1], in1=XC[:, o0:o1], op0=ALU.mult, op1=ALU.add),
                          reads=['RX', 'XC', 'sp_'], writes=['XC'])
                for d in range(2):
                    for t0 in range(0, TS, 512):
                        n = min(512, TS - t0)
                        pa = psbank(); pi = psbank()
                        E('pe', lambda e: e.matmul(ps[:, pa, :n], lw[:, d, :], XC[:, t0:t0 + n], start=True, stop=True), reads=['lw', 'XC'], writes=[PSK[pa]])
                        E('pe', lambda e: e.matmul(ps[:, pi, :n], lw[:, 2 + d, :], XC[:, t0:t0 + n], start=True, stop=True), reads=['lw', 'XC'], writes=[PSK[pi]])
                        E('act', lambda e: e.activation(out=A_[:, t0:t0 + n], in_=ps[:, pa, :n], func=AF.Sigmoid, bias=sp_[:, 5 + d:6 + d]), reads=[PSK[pa], 'sp_', 'A'], writes=['A'])
                        E('act', lambda e: e.activation(out=B_[:, t0:t0 + n], in_=ps[:, pi, :n], func=AF.Sigmoid, bias=sp_[:, 7 + d:8 + d]), reads=[PSK[pi], 'sp_', 'B'], writes=['B'])
                    E('act', lambda e: e.activation(out=A_[:], in_=A_[:], func=AF.Exp, scale=sl[:, 2 + d:3 + d]), reads=['A', 'sl'], writes=['A'])
                    E('dve', lambda e: e.tensor_tensor(out=T1[:], in0=A_[:], in1=A_[:], op=ALU.mult), reads=['A', 'T1'], writes=['T1'])
                    E('dve', lambda e: e.tensor_scalar(out=T1[:], in0=T1[:], scalar1=-1.0, scalar2=1.0, op0=ALU.mult, op1=ALU.add), reads=['T1'], writes=['T1'])
                    E('act', lambda e: e.activation(out=T1[:], in_=T1[:], func=AF.Sqrt), reads=['T1'], writes=['T1'])
                    E('dve', lambda e: e.tensor_tensor(out=B_[:], in0=B_[:], in1=XC[:], op=ALU.mult), reads=['B', 'XC'], writes=['B'])
                    E('dve', lambda e: e.tensor_tensor(out=B_[:], in0=B_[:], in1=T1[:], op=ALU.mult), reads=['B', 'T1'], writes=['B'])
                    if d == 0:
                        E('dve', lambda e: e.tensor_tensor_scan(out=HF[:], data0=A_[:], data1=B_[:], initial=0.0, op0=ALU.mult, op1=ALU.add), reads=['A', 'B', 'HF'], writes=['HF'])
                    else:
                        E('dve', lambda e: e.tensor_tensor_scan(out=HB[:, 0:CTX][:, ::-1], data0=A_[:, 0:CTX][:, ::-1], data1=B_[:, 0:CTX][:, ::-1], initial=0.0, op0=ALU.mult, op1=ALU.add),
                          reads=['A', 'B', 'HB'], writes=['HB'])
                        E('dve', lambda e: e.tensor_copy(out=hb0[:], in_=HB[:, 0:1]), reads=['HB'], writes=['hb0'])
                        E('dve', lambda e: e.tensor_tensor_scan(out=HB[:, CTX:TS][:, ::-1], data0=A_[:, CTX:TS][:, ::-1], data1=B_[:, CTX:TS][:, ::-1], initial=hb0[:, 0:1], op0=ALU.mult, op1=ALU.add),
                          reads=['A', 'B', 'HB', 'hb0'], writes=['HB'])
                E('dve', lambda e: e.tensor_tensor(out=T1[:], in0=RG[:], in1=RG[:], op=ALU.mult), reads=['RG', 'T1'], writes=['T1'])
                E('dve', lambda e: e.tensor_scalar(out=T1[:], in0=T1[:], scalar1=0.044715, scalar2=1.0, op0=ALU.mult, op1=ALU.add), reads=['T1'], writes=['T1'])
                E('dve', lambda e: e.tensor_tensor(out=T1[:], in0=T1[:], in1=RG[:], op=ALU.mult), reads=['T1', 'RG'], writes=['T1'])
                E('act', lambda e: e.activation(out=T1[:], in_=T1[:], func=AF.Sigmoid, scale=1.5957691216057308), reads=['T1'], writes=['T1'])
                E('dve', lambda e: e.tensor_tensor(out=T1[:], in0=T1[:], in1=RG[:], op=ALU.mult), reads=['T1', 'RG'], writes=['T1'])
                E('dve', lambda e: e.tensor_tensor(out=HF[:], in0=HF[:], in1=HB[:], op=ALU.add), reads=['HF', 'HB'], writes=['HF'])
                E('dve', lambda e: e.tensor_tensor(out=MX[:], in0=HF[:], in1=T1[:], op=ALU.mult), reads=['HF', 'T1', 'MX'], writes=['MX'])
                E('sp', lambda e: e.dma_start(out=mixT[b, 4 * 128:5 * 128, :], in_=MX[:]), reads=['MX'], writes=[])
            tr.fence()

    def phase_outproj(l):
        TT = 256
        with ExitStack() as es:
            W = sbt(es, "Wo", [128, 5, D], BF16)
            E('gq', lambda e: e.dma_start(out=W[:], in_=wout[l].rearrange("(s p) d -> p s d", p=128)), writes=['W'])
            mt = [sbt(es, "omt%d" % i, [128, 5, TT], BF16) for i in range(2)]
            st = [sbt(es, "ost%d" % i, [128, KH, TT]) for i in range(2)]
            it = 0
            for hs_ in range(NS):
                for r in range(NCORES):
                    b, p = r // 2, r % 2
                    for (s0, n, isctx) in ltiles(TT):
                        b2 = it % 2; it += 1
                        mk_, sk = ('omt', b2), ('ost', b2)
                        pos = seqpos(p, s0, isctx)
                        E('sp', lambda e: e.dma_start(out=mt[b2][:, :, :n], in_=mixT[b, :, pos:pos + n].rearrange("(s p) t -> p s t", p=128)), reads=[], writes=[mk_])
                        for dc in range(hs_ * KH, (hs_ + 1) * KH):
                            pb = psbank()
                            for s5 in range(5):
                                E('pe', lambda e: e.matmul(ps[:, pb, :n], W[:, s5, dc * 128:(dc + 1) * 128], mt[b2][:, s5, :n], start=(s5 == 0), stop=(s5 == 4)),
                                  reads=['W', mk_], writes=[PSK[pb]])
                            if dc % 2 == 0:
                                E('act', lambda e: e.activation(out=st[b2][:, dc - hs_ * KH, :n], in_=ps[:, pb, :n], func=AF.Copy), reads=[PSK[pb]], writes=[sk])
                            else:
                                E('dve', lambda e: e.tensor_copy(out=st[b2][:, dc - hs_ * KH, :n], in_=ps[:, pb, :n]), reads=[PSK[pb]], writes=[sk])
                        E('sp', lambda e: e.dma_start(out=H['part'][hs_][r * DH:(r + 1) * DH, s0:s0 + n].rearrange("(k p) t -> p k t", p=128), in_=st[b2][:, :, :n]), reads=[sk, ('part', hs_)], writes=[])
                if hs_ == 0 and NS == 2 and RS_OVERLAP:
                    reduce_scatter([0])
            tr.fence()

    def phase_ffn(w1, w3, w2, FSZ, gated):
        TT = 512
        FG = min(512, FSZ)
        NFK = FSZ // 128
        with ExitStack() as es:
            W1 = sbt(es, "fW1", [128, KD, FG], BF16); W3 = sbt(es, "fW3", [128, KD, FG], BF16)
            hT = [sbt(es, "fh%d" % i, [128, KD, TT], BF16) for i in range(2)]
            sa = [sbt(es, "fsa%d" % i, [128, TT]) for i in range(2)]
            hs = [sbt(es, "fhs%d" % i, [128, FG // 128, TT], BF16) for i in range(2)]
            it = 0; ai = 0
            for g0 in range(0, FSZ, FG):
                for k0 in range(0, KD, 4):
                    k1 = min(KD, k0 + 4)
                    E('gq', lambda e: e.dma_start(out=W1[:, k0:k1, :], in_=w1[k0 * 128:k1 * 128, g0:g0 + FG].rearrange("(k p) c -> p k c", p=128)), writes=['W1'])
                    E('gq', lambda e: e.dma_start(out=W3[:, k0:k1, :], in_=w3[k0 * 128:k1 * 128, g0:g0 + FG].rearrange("(k p) c -> p k c", p=128)), writes=['W3'])
                for r in range(NCORES):
                    for (s0, n, isctx) in ltiles(TT):
                        b2 = it % 2; it += 1
                        hk, sk = ('fh', b2), ('fhs', b2)
                        h_ = hT[b2]
                        for hs_ in range(NS):
                            E('gq', lambda e: e.dma_start(out=h_[:, hs_ * KH:(hs_ + 1) * KH, :n], in_=H['hfull'][hs_][r * DH:(r + 1) * DH, s0:s0 + n].rearrange("(k p) t -> p k t", p=128)), reads=['hfull'], writes=[hk])
                        for fc in range(FG // 128):
                            pa = psbank(); pb = psbank()
                            for k in range(KD):
                                E('pe', lambda e: e.matmul(ps[:, pa, :n], W1[:, k, fc * 128:(fc + 1) * 128], h_[:, k, :n], start=(k == 0), stop=(k == KD - 1)), reads=['W1', hk], writes=[PSK[pa]])
                            for k in range(KD):
                                E('pe', lambda e: e.matmul(ps[:, pb, :n], W3[:, k, fc * 128:(fc + 1) * 128], h_[:, k, :n], start=(k == 0), stop=(k == KD - 1)), reads=['W3', hk], writes=[PSK[pb]])
                            a2 = ai % 2; ai += 1
                            E('act', lambda e: e.activation(out=sa[a2][:, :n], in_=ps[:, pa, :n], func=AF.Silu), reads=[PSK[pa]], writes=[('fsa', a2)])
                            E('dve', lambda e: e.tensor_tensor(out=hs[b2][:, fc, :n], in0=sa[a2][:, :n], in1=ps[:, pb, :n], op=ALU.mult), reads=[('fsa', a2), PSK[pb]], writes=[sk])
                        E('sp', lambda e: e.dma_start(out=hid[r, g0:g0 + FG, s0:s0 + n].rearrange("(f p) t -> p f t", p=128), in_=hs[b2][:, :, :n]), reads=[sk], writes=[])
            tr.fence()
        with ExitStack() as es:
            DG = min(DH, max(512, (65536 // (NFK * 2)) // 128 * 128))
            W2 = sbt(es, "fW2", [128, NFK, DG], BF16)
            hd = [sbt(es, "fhd%d" % i, [128, NFK, TT], BF16) for i in range(2)]
            st = [sbt(es, "fst%d" % i, [128, DG // 128, TT]) for i in range(2)]
            gb = [sbt(es, "fgb%d" % i, [128, TT]) for i in range(2)] if gated else None
            it = 0
            for d0 in range(0, D, DG):
                for k0 in range(0, NFK, 4):
                    k1 = min(NFK, k0 + 4)
                    E('gq', lambda e: e.dma_start(out=W2[:, k0:k1, :], in_=w2[k0 * 128:k1 * 128, d0:d0 + DG].rearrange("(k p) c -> p k c", p=128)), writes=['W2'])
                for r in range(NCORES):
                    for (s0, n, isctx) in ltiles(TT):
                        b2 = it % 2; it += 1
                        hk, sk, gk = ('fhd', b2), ('fst', b2), ('fgb', b2)
                        E('sp', lambda e: e.dma_start(out=hd[b2][:, :, :n], in_=hid[r, 0:FSZ, s0:s0 + n].rearrange("(f p) t -> p f t", p=128)), reads=[], writes=[hk])
                        if gated:
                            E('sp', lambda e: e.dma_start(out=gb[b2][:, :n], in_=gmine[:, r * TL + s0:r * TL + s0 + n].partition_broadcast(128)), reads=[], writes=[gk])
                        for dc in range(DG // 128):
                            pb = psbank()
                            for fk in range(NFK):
                                E('pe', lambda e: e.matmul(ps[:, pb, :n], W2[:, fk, dc * 128:(dc + 1) * 128], hd[b2][:, fk, :n], start=(fk == 0), stop=(fk == NFK - 1)), reads=['W2', hk], writes=[PSK[pb]])
                            if gated:
                                E('dve', lambda e: e.tensor_tensor(out=st[b2][:, dc, :n], in0=ps[:, pb, :n], in1=gb[b2][:, :n], op=ALU.mult), reads=[PSK[pb], gk], writes=[sk])
                            elif dc % 2 == 0:
                                E('act', lambda e: e.activation(out=st[b2][:, dc, :n], in_=ps[:, pb, :n], func=AF.Copy), reads=[PSK[pb]], writes=[sk])
                            else:
                                E('dve', lambda e: e.tensor_copy(out=st[b2][:, dc, :n], in_=ps[:, pb, :n]), reads=[PSK[pb]], writes=[sk])
                        E('sp', lambda e: e.dma_start(out=H['part'][d0 // DH][r * DH + d0 % DH:r * DH + d0 % DH + DG, s0:s0 + n].rearrange("(k p) t -> p k t", p=128), in_=st[b2][:, :, :n]), reads=[sk, ('part', d0 // DH)], writes=[])
                if d0 + DG == DH and NS == 2 and RS_OVERLAP:
                    reduce_scatter([0])
            tr.fence()

    def phase_gates():
        if NOCC:
            fake_ag(gsh, gall, TL)
        else:
            E('cc', lambda e: e.collective_compute("AllGather", ALU.bypass, replica_groups=[list(range(NCORES))], ins=[gsh], outs=[gall]),
              reads=[], writes=['gall'])
        NT = NCORES * TL
        with ExitStack() as es:
            nfull = NT // 128
            ga = sbt(es, "ga", [128, nfull, 8]); gm = sbt(es, "gm", [128, nfull])
            E('sp', lambda e: e.dma_start(out=ga[:], in_=gall[0:nfull * 128, :].rearrange("(m p) x -> p m x", p=128)), reads=['gall'], writes=['ga'])
            E('dve', lambda e: e.tensor_tensor(out=ga[:], in0=ga[:], in1=sele_t[:].unsqueeze(1).broadcast_to([128, nfull, 8]), op=ALU.mult), reads=['ga', 'sele'], writes=['ga'])
            E('dve', lambda e: e.reduce_sum(out=gm[:], in_=ga[:], axis=AX.X), reads=['ga'], writes=['gm'])
            E('sp', lambda e: e.dma_start(out=gmine[0, 0:nfull * 128].rearrange("(m p) -> p m", p=128), in_=gm[:], allow_slow_non_contiguous=True), reads=['gm'], writes=[])
            tr.fence()

    import os as _os
    limit = int(_os.environ.get("KSTOP", "1000"))
    stepn = [0]

    def step(fn, *a, **kw):
        stepn[0] += 1
        if stepn[0] <= limit:
            fn(*a, **kw)
    step(phase_mod)

    def mv(l, t, i):
        return modv[:, l, t, i, :]

    def AB(l, w):
        return (Acoef[:, l, 0, w, :], Acoef[:, l, 1, w, :])
    for l in range(2):
        last = (l == 1)
        setuse(ag=2 * l)
        if l == 0:
            step(phase_tok, xT, None, None, None, AB(0, 0), (mv(0, 0, 0), mv(0, 1, 0)), h_out='H')
            xsrc = xT
        else:
            step(phase_tok, xsrc, 'O', (mv(0, 0, 5), mv(0, 1, 5)), xcur, AB(1, 0), (mv(1, 0, 0), mv(1, 1, 0)), h_out='H')
            xsrc = xcur
        step(allgather_h)
        step(phase_proj, l)
        step(phase_na, l, not last)
        step(phase_gla, l, not last)
        step(phase_lru, l)
        setuse(rs=2 * l)
        step(phase_outproj, l)
        step(reduce_scatter, [1] if RS_OVERLAP else None)
        setuse(ag=2 * l + 1)
        step(phase_tok, xsrc, 'O', (mv(l, 0, 2), mv(l, 1, 2)), xcur, AB(l, 1), (mv(l, 0, 3), mv(l, 1, 3)), h_out='H', router=last)
        xsrc = xcur
        step(allgather_h)
        setuse(rs=2 * l + 1)
        if not last:
            step(phase_ffn, w1d, w3d, w2d, cfg.FS, False)
        else:
            step(phase_gates)
            step(phase_ffn, w1e, w3e, w2e, cfg.FFE, True)
        step(reduce_scatter, [1] if RS_OVERLAP else None)
    step(phase_tok, xsrc, 'O', (mv(1, 0, 5), mv(1, 1, 5)), None, (nrm[:, 4, :], nrm[:, 4, :]), None, final=True)
    tr.fence()
    top.close()
    return nc


OFF = dict(naq=0, nak=2048, nav=4096, gq=6144, gk=6656, gv=7168, gg=8192, lrf=9216, lrb=9232, rx=9248, rg=10272)


def rope_tables(cfg):
    pos = np.arange(cfg.SEQ, dtype=np.int32)
    row, col = (pos // 64).astype(np.float32), (pos % 64).astype(np.float32)
    quarter = 32
    inv_freq = (np.float32(10000.0) ** (-np.arange(quarter, dtype=np.float32) / np.float32(quarter))).astype(np.float32)
    cosT = np.zeros((128, cfg.SEQ), np.float32); sinS = np.zeros((128, cfg.SEQ), np.float32)
    for p in range(128):
        pp = row if p < 64 else col
        ang = (pp * inv_freq[p % 32]).astype(np.float32)
        cosT[p] = np.cos(ang); s = np.sin(ang)
        sinS[p] = -s if (p % 64) < 32 else s
    return cosT, sinS


def na_bias_tables(rpb2, cfg):
    qc = np.arange(64); kc = np.arange(64)
    cs = np.clip(qc - 8, 0, 48)
    inwin = (kc[:, None] >= cs[None, :]) & (kc[:, None] < cs[None, :] + 16)
    dcol = np.clip(kc[:, None] - qc[None, :] + 15, 0, 30)
    out = np.full((2, 64, 2, 8, 4, 64), NEG, np.float32)
    for hh in range(2):
        for cf in range(8):
            for jj in range(4):
                for i2 in range(2):
                    dr = -cf + 2 * jj + i2 + 7
                    if 0 <= dr <= 14:
                        vals = rpb2[hh, dr][dcol]
                        out[i2, :, hh, cf, jj, :] = np.where(inwin, vals, np.float32(NEG))
    return np.ascontiguousarray(out.reshape(128, 2 * 8 * 4 * 64))


def prepare(cfg, I):
    D, CH, LH, MC, FS = cfg.D, cfg.CH, cfg.LH, cfg.MC, cfg.FS
    f = lambda a: np.ascontiguousarray(a, dtype=np.float32)
    cosT, sinS = rope_tables(cfg)
    c5T = f(np.concatenate([I['c'], I['c_ctx'][None]], 0).T.reshape(cfg.KD, 128, 5).transpose(1, 0, 2))
    normp = f(np.stack([I['norm_mix'][0], I['norm_mix'][1], I['norm_ffn'][0], I['norm_ffn'][1], I['final_norm']], 0).reshape(5, cfg.KD, 128).transpose(2, 0, 1))
    tri = np.zeros((2, 64, 64), np.float32)
    jj, ii = np.meshgrid(np.arange(64), np.arange(64), indexing='ij')
    tri[0] = (ii >= jj); tri[1] = (ii <= jj)
    chm = np.ones((1, cfg.TS), np.float32); chm[0, ::64] = 0.0
    ident = np.eye(128, dtype=np.float32)
    perm = np.arange(128) ^ 32
    maps = []
    for c in range(NCORES):
        b, p = c // 2, c % 2
        hg, hp = c // 2, c % 2
        m = {}
        m['xT'] = f(np.concatenate([I['ctx'][b, p * CH:(p + 1) * CH], I['x'][b, p * LH:(p + 1) * LH]], 0).T)
        m['c5T'] = c5T
        m['wmod'] = f(I['w_mod'][:, :, c * MC:(c + 1) * MC])
        m['bmod'] = f(I['b_mod'][:, c * MC:(c + 1) * MC].reshape(2, cfg.MJ, 128).transpose(2, 0, 1))
        m['normp'] = normp
        sb_ = np.zeros((128, 4), np.float32); sb_[:, b] = 1.0; m['selb'] = sb_
        se_ = np.zeros((128, 8), np.float32); se_[:, c] = 1.0; m['sele'] = se_
        wi = I['w_in']
        gq = wi[:, :, OFF['gq'] + hg * 128:OFF['gq'] + (hg + 1) * 128]
        gk = wi[:, :, OFF['gk'] + hg * 128:OFF['gk'] + (hg + 1) * 128]
        cols = [wi[:, :, OFF['naq'] + (2 * c) * 128:OFF['naq'] + (2 * c + 2) * 128],
                wi[:, :, OFF['nak'] + (2 * c) * 128:OFF['nak'] + (2 * c + 2) * 128],
                gq, gk, gq[:, :, perm], gk[:, :, perm],
                wi[:, :, OFF['lrf']:OFF['lrf'] + 32],
                wi[:, :, OFF['rx'] + c * 128:OFF['rx'] + (c + 1) * 128],
                wi[:, :, OFF['rg'] + c * 128:OFF['rg'] + (c + 1) * 128]]
        lrpad = np.zeros((2, D, 96), np.float32)
        wf = np.concatenate(cols[:7] + [lrpad] + cols[7:], axis=2)
        m['wfm'] = f(wf)
        m['wtm'] = f(np.concatenate([wi[:, :, OFF['nav'] + (2 * c) * 128:OFF['nav'] + (2 * c + 2) * 128],
                                     wi[:, :, OFF['gv'] + hg * 256:OFF['gv'] + (hg + 1) * 256],
                                     wi[:, :, OFF['gg'] + hg * 256:OFF['gg'] + (hg + 1) * 256]], axis=2))
        m['ropec'] = cosT; m['ropes'] = sinS
        m['nabi'] = np.stack([na_bias_tables(I['na_rpb'][l, 2 * c:2 * c + 2], cfg) for l in range(2)], 0)
        wgp = np.zeros((2, 2, 32, 128), np.float32)
        for d in range(2):
            wgp[:, d, d * 16:(d + 1) * 16, :] = I['gla_wg'][:, d, :, hg * 128:(hg + 1) * 128]
        m['wgp'] = wgp
        m['gnorm'] = f(I['gla_norm'][:, None, :])
        sp = np.zeros((2, 128, 16), np.float32)
        blk = slice(c * 128, (c + 1) * 128)
        sp[:, :, 0:4] = np.transpose(I['conv_w'][:, :, blk], (0, 2, 1))
        sp[:, :, 4] = I['conv_b'][:, blk]
        sp[:, :, 5:7] = np.transpose(I['lru_ba'][:, :, blk], (0, 2, 1))
        sp[:, :, 7:9] = np.transpose(I['lru_bx'][:, :, blk], (0, 2, 1))
        sp[:, :, 9:11] = np.transpose(I['lru_lam'][:, :, blk], (0, 2, 1))
        sp[:, :, 11:13] = np.transpose(I['gla_bg'][:, :, hg * 128:(hg + 1) * 128], (0, 2, 1))
        m['smallp'] = sp
        m['lruw'] = f(np.concatenate([I['lru_wa'][:, :, c], I['lru_wx'][:, :, c]], axis=1))
        wo = I['w_out']
        gl = wo[:, 2048 + hg * 256:2048 + (hg + 1) * 256, :].copy()
        gl[:, (1 - hp) * 128:(2 - hp) * 128, :] = 0.0
        m['wout'] = f(np.concatenate([wo[:, (2 * c) * 128:(2 * c + 2) * 128, :], gl, wo[:, 3072 + c * 128:3072 + (c + 1) * 128, :]], axis=1))
        m['w1d'] = f(I['ffd_w1'][0][:, c * FS:(c + 1) * FS]); m['w3d'] = f(I['ffd_w3'][0][:, c * FS:(c + 1) * FS])
        m['w2d'] = f(I['ffd_w2'][0][c * FS:(c + 1) * FS, :])
        m['w1e'] = f(I['moe_w1'][0][c]); m['w3e'] = f(I['moe_w3'][0][c]); m['w2e'] = f(I['moe_w2'][0][c])
        m['rtr'] = f(I['router'][0].reshape(cfg.KD, 128, 8).transpose(1, 0, 2))
        m['trimask'] = tri; m['chmask'] = chm; m['identb'] = ident
        maps.append(m)
    return maps


def run(cfg, inputs, dbg=()):
    I = {k: np.asarray(v) for k, v in inputs.items()}
    nc = build(cfg, dbg)
    maps = prepare(cfg, I)
    res = run_bass_kernel_spmd(nc, maps, core_ids=list(range(NCORES)))
    out = np.zeros((4, cfg.SEQ, cfg.D), np.float32)
    for c in range(NCORES):
        b, p = c // 2, c % 2
        out[b, p * cfg.LH:(p + 1) * cfg.LH, :] = res.results[c]["outT"].T
    return out, res


def kernel(**inputs):
    out, _ = run(FULL, inputs)
    return out
```
